# Optimizing a Trainium2 kernel written in Bass

```python
import jax
import jax.numpy as jnp
from jax import lax
import numpy as np

D_MODEL = 1024
BATCH = 16
SEQ = 2048
DEPTH = 2

MEM_LEN = 256
EPS = 1e-6
FOX_HEADS = 8
FOX_HEAD_DIM = 64
FOX_WIDTH = FOX_HEADS * FOX_HEAD_DIM
FOX_BLOCK = 128
FORGET_BIAS_INIT = 3.0
GLA_HEADS = 4
GLA_KEY_DIM = 64
GLA_VAL_DIM = 128
GLA_QK_WIDTH = GLA_HEADS * GLA_KEY_DIM
GLA_V_WIDTH = GLA_HEADS * GLA_VAL_DIM
GLA_GATE_RANK = 16
GLA_GATE_TAU = 16.0
GLA_CHUNK = 64
MIX_WIDTH = FOX_WIDTH + GLA_V_WIDTH
IN_WIDTHS = (FOX_WIDTH, FOX_WIDTH, FOX_WIDTH, FOX_HEADS, GLA_QK_WIDTH, GLA_QK_WIDTH, GLA_V_WIDTH, GLA_V_WIDTH, GLA_GATE_RANK)
IN_PROJ_WIDTH = 3096
X_HEADS = 4
X_HEAD_DIM = 128
X_WIDTH = X_HEADS * X_HEAD_DIM
N_GROUPS = 4
EXPERTS_PER_GROUP = 4
N_EXPERTS = N_GROUPS * EXPERTS_PER_GROUP
TOP_K = 2
D_EXPERT = 512
MOE_BLOCK = 128
RESID_SCALE = 0.5

kernel_name = 'hybrid_fox_gla_hmoe_block'


def rms_norm(x, gain):
    xf = x.astype(jnp.float32)
    y = xf * lax.rsqrt(jnp.mean(xf * xf, axis=-1, keepdims=True) + EPS)
    return (y * gain.astype(jnp.float32)).astype(x.dtype)


def forgetting_attention(q, k, v, f_logit):
    B, S, H, d = q.shape
    q = q.transpose(0, 2, 1, 3)
    k = k.transpose(0, 2, 1, 3)
    v = v.transpose(0, 2, 1, 3)
    log_f = jax.nn.log_sigmoid(f_logit.astype(jnp.float32)).transpose(0, 2, 1)
    c = jnp.cumsum(log_f, axis=-1)
    scale = d ** -0.5
    outs = []
    for blk in range(S // FOX_BLOCK):
        q0 = blk * FOX_BLOCK
        q1 = q0 + FOX_BLOCK
        s = jnp.einsum('bhqd,bhkd->bhqk', q[:, :, q0:q1], k[:, :, :q1], preferred_element_type=jnp.float32) * scale
        s = s + c[:, :, q0:q1, None] - c[:, :, None, :q1]
        causal = jnp.arange(q0, q1)[:, None] >= jnp.arange(q1)[None, :]
        p = jax.nn.softmax(jnp.where(causal, s, -jnp.inf), axis=-1)
        outs.append(jnp.einsum('bhqk,bhkd->bhqd', p.astype(v.dtype), v[:, :, :q1]))
    return jnp.concatenate(outs, axis=2).transpose(0, 2, 1, 3)


def gla_chunked(q, k, v, log_a):
    B, S, H, dk = q.shape
    dv = v.shape[-1]
    C = GLA_CHUNK
    nc = S // C

    def to_chunks(t):
        return t.astype(jnp.float32).reshape(B, nc, C, H, t.shape[-1]).transpose(1, 0, 3, 2, 4)

    qc = to_chunks(q) * (dk ** -0.5)
    kc = to_chunks(k)
    vc = to_chunks(v)
    gc = to_chunks(log_a)
    causal = jnp.tril(jnp.ones((C, C), dtype=bool))[:, :, None]

    def step(state, inp):
        qi, ki, vi, gi = inp
        b = jnp.cumsum(gi, axis=2)
        b_last = b[:, :, -1:, :]
        o_inter = jnp.einsum('bhtd,bhdv->bhtv', qi * jnp.exp(b), state)
        diff = b[:, :, :, None, :] - b[:, :, None, :, :]
        decay = jnp.exp(jnp.where(causal, diff, -jnp.inf))
        attn = jnp.einsum('bhtd,bhsd,bhtsd->bhts', qi, ki, decay)
        o_intra = jnp.einsum('bhts,bhsv->bhtv', attn, vi)
        new_state = jnp.exp(b_last[:, :, 0, :])[..., None] * state + jnp.einsum('bhsd,bhsv->bhdv', ki * jnp.exp(b_last - b), vi)
        return new_state, o_inter + o_intra

    state0 = jnp.zeros((B, H, dk, dv), jnp.float32)
    _, o = lax.scan(step, state0, (qc, kc, vc, gc))
    return o.transpose(1, 0, 3, 2, 4).reshape(B, S, H, dv).astype(v.dtype)


def hybrid_mixer(h, w_in, b_forget, w_alpha_up, b_alpha, fox_out_gain, gla_out_gain, w_out):
    B, S, _ = h.shape
    offsets = [int(o) for o in np.cumsum(IN_WIDTHS)[:-1]]
    fq, fk, fv, ff, gq, gk, gv, gg, ga = jnp.split(h @ w_in, offsets, axis=-1)
    fox = forgetting_attention(
        fq.reshape(B, S, FOX_HEADS, FOX_HEAD_DIM),
        fk.reshape(B, S, FOX_HEADS, FOX_HEAD_DIM),
        fv.reshape(B, S, FOX_HEADS, FOX_HEAD_DIM),
        ff + b_forget)
    fox = rms_norm(fox, fox_out_gain.reshape(FOX_HEADS, FOX_HEAD_DIM))
    log_a = jax.nn.log_sigmoid((ga @ w_alpha_up + b_alpha).astype(jnp.float32)) / GLA_GATE_TAU
    gla = gla_chunked(
        gq.reshape(B, S, GLA_HEADS, GLA_KEY_DIM),
        gk.reshape(B, S, GLA_HEADS, GLA_KEY_DIM),
        gv.reshape(B, S, GLA_HEADS, GLA_VAL_DIM),
        log_a.reshape(B, S, GLA_HEADS, GLA_KEY_DIM))
    gla = rms_norm(gla, gla_out_gain.reshape(GLA_HEADS, GLA_VAL_DIM)) * jax.nn.silu(gg.reshape(B, S, GLA_HEADS, GLA_VAL_DIM))
    y = jnp.concatenate([fox.reshape(B, S, FOX_WIDTH), gla.reshape(B, S, GLA_V_WIDTH)], axis=-1)
    return y @ w_out


def memory_cross_attention(h, mem_n, w_xq, w_xk, w_xv, w_xo):
    B, S, _ = h.shape
    M = mem_n.shape[1]
    q = (h @ w_xq).reshape(B, S, X_HEADS, X_HEAD_DIM)
    k = (mem_n @ w_xk).reshape(B, M, X_HEADS, X_HEAD_DIM)
    v = (mem_n @ w_xv).reshape(B, M, X_HEADS, X_HEAD_DIM)
    s = jnp.einsum('bqhd,bkhd->bhqk', q, k, preferred_element_type=jnp.float32) * (X_HEAD_DIM ** -0.5)
    p = jax.nn.softmax(s, axis=-1)
    o = jnp.einsum('bhqk,bkhd->bqhd', p.astype(v.dtype), v).reshape(B, S, X_WIDTH)
    return o @ w_xo


def hierarchical_moe(h, w_rg, b_rg, w_re, b_re, w_e_gate, w_e_up, w_e_down):
    B, S, D = h.shape
    N = B * S
    hf = h.reshape(N, D)
    g_prob = jax.nn.softmax((hf @ w_rg).astype(jnp.float32) + b_rg.astype(jnp.float32), axis=-1)
    grp = jnp.argmax(g_prob, axis=-1)
    p_grp = jnp.take_along_axis(g_prob, grp[:, None], axis=-1)
    e_logits = ((hf @ w_re).astype(jnp.float32) + b_re.astype(jnp.float32)).reshape(N, N_GROUPS, EXPERTS_PER_GROUP)
    e_logits = jnp.take_along_axis(e_logits, grp[:, None, None], axis=1)[:, 0]
    top_p, top_i = lax.top_k(jax.nn.softmax(e_logits, axis=-1), TOP_K)
    gate = p_grp * top_p / jnp.sum(top_p, axis=-1, keepdims=True)
    expert = grp[:, None].astype(jnp.int32) * EXPERTS_PER_GROUP + top_i.astype(jnp.int32)
    Mrows = N * TOP_K
    flat_e = expert.reshape(Mrows)
    flat_g = gate.reshape(Mrows)
    flat_tok = jnp.repeat(jnp.arange(N, dtype=jnp.int32), TOP_K)
    order = jnp.argsort(flat_e)
    sorted_e = flat_e[order]
    counts = jnp.bincount(flat_e, length=N_EXPERTS)
    padded = (counts + MOE_BLOCK - 1) // MOE_BLOCK * MOE_BLOCK
    starts = jnp.cumsum(counts) - counts
    pad_ends = jnp.cumsum(padded)
    pad_starts = pad_ends - padded
    dest = pad_starts[sorted_e] + jnp.arange(Mrows, dtype=jnp.int32) - starts[sorted_e]
    R = Mrows + N_EXPERTS * MOE_BLOCK
    n_blocks = R // MOE_BLOCK
    row_tok = jnp.full((R,), N, dtype=jnp.int32).at[dest].set(flat_tok[order])
    row_gate = jnp.zeros((R,), jnp.float32).at[dest].set(flat_g[order])
    block_e = jnp.minimum(jnp.searchsorted(pad_ends, jnp.arange(n_blocks) * MOE_BLOCK, side='right'), N_EXPERTS - 1)
    x_rows = jnp.concatenate([hf, jnp.zeros((1, D), hf.dtype)], axis=0)[row_tok].reshape(n_blocks, MOE_BLOCK, D)

    def expert_block(args):
        xb, e = args
        u = jax.nn.silu(xb @ w_e_gate[e]) * (xb @ w_e_up[e])
        return u @ w_e_down[e]

    y_rows = lax.map(expert_block, (x_rows, block_e)).reshape(R, D)
    y = jax.ops.segment_sum(y_rows.astype(jnp.float32) * row_gate[:, None], row_tok, num_segments=N + 1)[:N]
    return y.reshape(B, S, D).astype(h.dtype)


def setup_inputs(seed: int = 0) -> dict:
    key = jax.random.key(seed)
    ks = jax.random.split(key, 26)
    f32 = jnp.float32
    L = DEPTH

    def w(k, shape, fan_in, scale=1.0):
        return jax.random.normal(k, shape, f32) * (scale * fan_in ** -0.5)

    def gain(k, shape):
        return 1.0 + 0.02 * jax.random.normal(k, shape, f32)

    def small(k, shape, scale):
        return scale * jax.random.normal(k, shape, f32)

    return {
        'x': jax.random.normal(ks[0], (BATCH, SEQ, D_MODEL), f32),
        'mem': jax.random.normal(ks[1], (BATCH, MEM_LEN, D_MODEL), f32),
        'mem_norm': gain(ks[2], (D_MODEL,)),
        'mix_norm': gain(ks[3], (L, D_MODEL)),
        'w_in': w(ks[4], (L, D_MODEL, IN_PROJ_WIDTH), D_MODEL),
        'b_forget': FORGET_BIAS_INIT + small(ks[5], (L, FOX_HEADS), 0.1),
        'w_alpha_up': w(ks[6], (L, GLA_GATE_RANK, GLA_QK_WIDTH), GLA_GATE_RANK),
        'b_alpha': small(ks[7], (L, GLA_QK_WIDTH), 0.1),
        'fox_out_gain': gain(ks[8], (L, FOX_WIDTH)),
        'gla_out_gain': gain(ks[9], (L, GLA_V_WIDTH)),
        'w_out': w(ks[10], (L, MIX_WIDTH, D_MODEL), MIX_WIDTH, RESID_SCALE),
        'cross_norm': gain(ks[11], (L, D_MODEL)),
        'w_xq': w(ks[12], (L, D_MODEL, X_WIDTH), D_MODEL),
        'w_xk': w(ks[13], (L, D_MODEL, X_WIDTH), D_MODEL),
        'w_xv': w(ks[14], (L, D_MODEL, X_WIDTH), D_MODEL),
        'w_xo': w(ks[15], (L, X_WIDTH, D_MODEL), X_WIDTH, RESID_SCALE),
        'moe_norm': gain(ks[16], (L, D_MODEL)),
        'w_router_group': w(ks[17], (L, D_MODEL, N_GROUPS), D_MODEL),
        'b_router_group': small(ks[18], (L, N_GROUPS), 0.01),
        'w_router_expert': w(ks[19], (L, D_MODEL, N_EXPERTS), D_MODEL),
        'b_router_expert': small(ks[20], (L, N_EXPERTS), 0.01),
        'w_expert_gate': w(ks[21], (L, N_EXPERTS, D_MODEL, D_EXPERT), D_MODEL),
        'w_expert_up': w(ks[22], (L, N_EXPERTS, D_MODEL, D_EXPERT), D_MODEL),
        'w_expert_down': w(ks[23], (L, N_EXPERTS, D_EXPERT, D_MODEL), D_EXPERT, RESID_SCALE),
        'final_norm': gain(ks[24], (D_MODEL,)),
    }


def reference(x, mem, mem_norm, mix_norm, w_in, b_forget, w_alpha_up, b_alpha, fox_out_gain, gla_out_gain, w_out,
              cross_norm, w_xq, w_xk, w_xv, w_xo, moe_norm, w_router_group, b_router_group, w_router_expert,
              b_router_expert, w_expert_gate, w_expert_up, w_expert_down, final_norm):
    mem_n = rms_norm(mem, mem_norm)
    h = x
    for l in range(DEPTH):
        h = h + hybrid_mixer(rms_norm(h, mix_norm[l]), w_in[l], b_forget[l], w_alpha_up[l], b_alpha[l],
                             fox_out_gain[l], gla_out_gain[l], w_out[l])
        h = h + memory_cross_attention(rms_norm(h, cross_norm[l]), mem_n, w_xq[l], w_xk[l], w_xv[l], w_xo[l])
        h = h + hierarchical_moe(rms_norm(h, moe_norm[l]), w_router_group[l], b_router_group[l],
                                 w_router_expert[l], b_router_expert[l], w_expert_gate[l], w_expert_up[l],
                                 w_expert_down[l])
    return rms_norm(h, final_norm)
```

```python
import contextlib
import numpy as np
import ml_dtypes
import concourse.bass as bass
import concourse.mybir as mybir
from concourse.bass_utils import run_bass_kernel_spmd

F32 = mybir.dt.float32
BF16 = mybir.dt.bfloat16
AF = mybir.ActivationFunctionType
ALU = mybir.AluOpType
AX = mybir.AxisListType

SAME_RAW = True
SEM_ROT = 30000

DM = 1024
MEM = 256
EPS = 1e-6
O_FQ, O_FK, O_FV, O_FF, O_GQ, O_GK, O_GV, O_GG, O_GA, O_END = 0, 512, 1024, 1536, 1544, 1800, 2056, 2568, 3080, 3096
NEXP = 16
DEXP = 512
NEG = -30000.0


class Buf:
    __slots__ = ("name", "w", "r")

    def __init__(self, name=""):
        self.name = name
        self.w = None
        self.r = {}


class Tile:
    def __init__(self, t, name=""):
        self.t = t
        self.b = Buf(name)

    def __getitem__(self, k):
        return self.t[k]


class _Queue:
    def __init__(self, name):
        self.name = name
        self.items = []
        self.sem = None
        self.cnt = 0
        self.semidx = 0
        self.seen = {}
        self.pending = False


class Prog:
    def __init__(self, nc, stack, ndma=8):
        self.nc = nc
        self.stack = stack
        self.q = {n: _Queue(n) for n in ("pe", "act", "dve", "pool", "sp")}
        for q in self.q.values():
            self._newsem(q)
        self.dsem = {}
        for qn in ("sp", "pool", "act"):
            sems = [stack.enter_context(nc.semaphore(f"d_{qn}_{i}")) for i in range(ndma)]
            self.dsem[qn] = {"sems": sems, "cnt": [0] * ndma, "idx": 0}

    def _newsem(self, q):
        q.sem = self.stack.enter_context(self.nc.semaphore(f"s_{q.name}_{q.semidx}"))
        q.semidx += 1
        q.cnt = 0

    def _deps(self, q, qn, reads, writes, is_dma):
        toks = []
        for b in reads:
            if b.w is not None:
                toks.append((b.w, "raw"))
        for b in writes:
            if b.w is not None:
                toks.append((b.w, "waw"))
            for t in b.r.values():
                toks.append((t, "war"))
        waits = []
        for (sem, cnt, src), kind in toks:
            if (not is_dma) and src == qn:
                if qn == "pe":
                    continue
                if not SAME_RAW:
                    continue
            if q.seen.get(sem, 0) >= cnt:
                continue
            q.seen[sem] = cnt
            waits.append((sem, cnt))
        return waits

    def _mark(self, tok, reads, writes):
        for b in writes:
            b.w = tok
            b.r = {}
        for b in reads:
            if b not in writes:
                b.r[tok[0]] = tok

    def op(self, qn, fn, reads=(), writes=(), signal=True):
        q = self.q[qn]
        waits = self._deps(q, qn, reads, writes, False)
        if signal:
            if q.cnt >= SEM_ROT and not q.pending:
                self._newsem(q)
            q.cnt += 1
            tok = (q.sem, q.cnt, qn)
            q.pending = False
            q.items.append((waits, fn, ("inc", q.sem)))
        else:
            tok = (q.sem, q.cnt + 1, qn)
            q.pending = True
            q.items.append((waits, fn, None))
        self._mark(tok, reads, writes)
        return tok

    def dma(self, qn, out, in_, reads=(), writes=(), **kw):
        q = self.q[qn]
        pool = self.dsem[qn]
        i = pool["idx"]
        pool["idx"] = (i + 1) % len(pool["sems"])
        sem = pool["sems"][i]
        prev = pool["cnt"][i]
        waits = self._deps(q, qn, reads, writes, True)
        if prev > 0 and q.seen.get(sem, 0) < prev:
            q.seen[sem] = prev
            waits.append((sem, prev))
        pool["cnt"][i] = prev + 16
        tok = (sem, prev + 16, qn + "_dma")
        q.items.append((waits, (lambda h: h.dma_start(out=out, in_=in_, **kw)), ("dma", sem)))
        self._mark(tok, reads, writes)
        return tok

    def barrier(self):
        toks = []
        for qn, q in self.q.items():
            if q.pending:
                raise RuntimeError("barrier with pending unsignaled op on " + qn)
            if q.cnt > 0:
                toks.append((q.sem, q.cnt, qn))
        for qn, pool in self.dsem.items():
            for s, c in zip(pool["sems"], pool["cnt"]):
                if c > 0:
                    toks.append((s, c, qn + "_dma"))
        for qn, q in self.q.items():
            waits = []
            for sem, cnt, src in toks:
                if src == qn:
                    continue
                if q.seen.get(sem, 0) >= cnt:
                    continue
                q.seen[sem] = cnt
                waits.append((sem, cnt))
            if waits:
                q.items.append((waits, None, None))

    def emit(self):
        self.barrier()
        nc = self.nc

        def runner(q):
            def f(h):
                for waits, fn, sig in q.items:
                    for sem, cnt in waits:
                        h.wait_ge(sem, cnt)
                    if fn is None:
                        continue
                    ins = fn(h)
                    if sig is None:
                        continue
                    if sig[0] == "dma":
                        ins.then_inc(sig[1], 16)
                    else:
                        ins.then_inc(sig[1], 1)
            return f

        with nc.Block() as block:
            block.tensor(runner(self.q["pe"]))
            block.scalar(runner(self.q["act"]))
            block.vector(runner(self.q["dve"]))
            block.gpsimd(runner(self.q["pool"]))
            block.sync(runner(self.q["sp"]))

    def stats(self):
        return {n: len(q.items) for n, q in self.q.items()}


class Ctx:
    pass


def build(cfg):
    S = cfg["S"]
    NSEQ = cfg["NSEQ"]
    L = cfg["L"]
    phases = cfg.get("phases", "ABCDE")
    debug = cfg.get("debug", False)
    T = S * NSEQ
    NT = S // 128
    NG = S // 512
    nc = bass.Bass("TRN2", target_bir_lowering=False)
    C = Ctx()
    C.nc, C.S, C.NSEQ, C.L, C.T, C.NT, C.NG = nc, S, NSEQ, L, T, NT, NG
    C.cfg = cfg

    def din(name, shape, dt=F32):
        return nc.dram_tensor(name, list(shape), dt, kind="ExternalInput").ap()

    def dscr(name, shape, dt):
        return nc.dram_tensor(name, list(shape), dt, kind=("ExternalOutput" if debug else "Internal")).ap()

    I = {}
    I["x"] = din("x", [T, DM])
    I["mem"] = din("mem", [NSEQ * MEM, DM])
    I["mem_norm"] = din("mem_norm", [DM])
    I["mix_norm"] = din("mix_norm", [L, DM])
    I["w_in"] = din("w_in", [L, DM, O_END])
    I["b_forget"] = din("b_forget", [L, 8])
    I["w_alpha_up"] = din("w_alpha_up", [L, 16, 256])
    I["b_alpha"] = din("b_alpha", [L, 256])
    I["fox_out_gain"] = din("fox_out_gain", [L, 512])
    I["gla_out_gain"] = din("gla_out_gain", [L, 512])
    I["w_out"] = din("w_out", [L, DM, DM])
    I["cross_norm"] = din("cross_norm", [L, DM])
    I["w_xq"] = din("w_xq", [L, DM, 512])
    I["w_xk"] = din("w_xk", [L, DM, 512])
    I["w_xv"] = din("w_xv", [L, DM, 512])
    I["w_xo"] = din("w_xo", [L, 512, DM])
    I["moe_norm"] = din("moe_norm", [L, DM])
    I["w_rg"] = din("w_router_group", [L, DM, 4])
    I["b_rg"] = din("b_router_group", [L, 4])
    I["w_re"] = din("w_router_expert", [L, DM, 16])
    I["b_re"] = din("b_router_expert", [L, 16])
    I["w_eg"] = din("w_expert_gate", [L, NEXP, DM, DEXP])
    I["w_eu"] = din("w_expert_up", [L, NEXP, DM, DEXP])
    I["w_ed"] = din("w_expert_down", [L, NEXP, DEXP, DM])
    I["final_norm"] = din("final_norm", [DM])
    I["c_ident"] = din("c_ident", [128, 128])
    I["c_maskadd"] = din("c_maskadd", [128, 128])
    I["c_mask01"] = din("c_mask01", [128, 128])
    C.I = I
    out = nc.dram_tensor("y_out", [T, DM], F32, kind="ExternalOutput").ap()
    C.out = out

    D_ = {}
    D_["h"] = dscr("s_h", [T, DM], F32)
    D_["h2"] = dscr("s_h2", [T, DM], F32)
    C.hcur = I["x"]
    D_["QT"] = dscr("s_QT", [NSEQ, 4, 128, S], BF16)
    D_["KT"] = dscr("s_KT", [NSEQ, 4, 128, S], BF16)
    D_["vaug"] = dscr("s_vaug", [T, 520], BF16)
    D_["A"] = dscr("s_A", [NSEQ, 8, 2, S], F32)
    D_["B"] = dscr("s_B", [NSEQ, 8, 2, S], F32)
    D_["qgT"] = dscr("s_qgT", [NSEQ, 64, 4, S], BF16)
    D_["kgT"] = dscr("s_kgT", [NSEQ, 64, 4, S], BF16)
    D_["ktok"] = dscr("s_ktok", [T, 256], BF16)
    D_["gv"] = dscr("s_gv", [T, 512], BF16)
    D_["gg"] = dscr("s_gg", [T, 512], BF16)
    D_["ebl"] = dscr("s_ebl", [NSEQ, 64, 4, NT], F32)
    D_["ymix"] = dscr("s_ymix", [T, DM], BF16)
    C.D = D_

    with contextlib.ExitStack() as st:
        P = Prog(nc, st)
        C.P = P

        def gsb(name, shape, dt):
            return Tile(st.enter_context(nc.sbuf_tensor("g_" + name, shape, dt)), name)

        C.banks = [Tile(st.enter_context(nc.psum_tensor(f"bank{i}", [128, 512], F32)), f"bank{i}") for i in range(8)]
        C.bank_i = 0
        C.idf = gsb("idf", [128, 128], F32)
        C.idb = gsb("idb", [128, 128], BF16)
        C.maskadd = gsb("maskadd", [128, 128], BF16)
        C.mask01 = gsb("mask01", [128, 128], F32)
        C.ones = gsb("ones", [128, 512], F32)
        C.memnT = gsb("memnT", [128, 8, NSEQ * MEM], BF16)
        tmpc = gsb("tmpc", [128, 128], F32)
        P.dma("sp", C.idf[:], I["c_ident"], writes=[C.idf.b])
        P.op("act", lambda h: h.copy(out=C.idb[:], in_=C.idf[:]), reads=[C.idf.b], writes=[C.idb.b])
        P.dma("sp", tmpc[:], I["c_maskadd"], writes=[tmpc.b])
        P.op("act", lambda h: h.copy(out=C.maskadd[:], in_=tmpc[:]), reads=[tmpc.b], writes=[C.maskadd.b])
        P.dma("sp", C.mask01[:], I["c_mask01"], writes=[C.mask01.b])
        P.op("dve", lambda h: h.memset(C.ones[:], 1.0), writes=[C.ones.b])

        if "0" in phases:
            phase_mem(C)
        for l in range(L):
            if "A" in phases:
                phase_A(C, l)
            if "B" in phases or "1" in phases:
                phase_B1(C, l)
            if "B" in phases or "2" in phases:
                phase_B2(C, l)
            if "B" in phases or "3" in phases:
                phase_B3(C, l)
            if "C" in phases:
                phase_C(C, l)
            if "D" in phases:
                phase_D(C, l, last=(l == L - 1))
        P.emit()
        C.stats = P.stats()
    return nc, C


def next_bank(C):
    b = C.banks[C.bank_i]
    C.bank_i = (C.bank_i + 1) % 8
    return b


def bf16view(bank):
    return bank.t[:].bitcast(BF16)


def MM(C, out, lhsT, rhs, start, stop, reads, writes, signal=True):
    C.P.op("pe", lambda h: h.matmul(out=out, lhsT=lhsT, rhs=rhs, start=start, stop=stop), reads, writes, signal)


def TR(C, out, in_, ident, reads, writes, signal=True):
    C.P.op("pe", lambda h: h.transpose(out=out, in_=in_, identity=ident), reads, writes, signal)


def ACT(C, out, in_, func, reads, writes, **kw):
    C.P.op("act", lambda h: h.activation(out=out, in_=in_, func=func, **kw), reads, writes)


def TT(C, out, in0, in1, op, reads, writes):
    C.P.op("dve", lambda h: h.tensor_tensor(out=out, in0=in0, in1=in1, op=op), reads, writes)


def TS(C, out, in0, s1, s2, op0, op1, reads, writes):
    if s2 is None:
        C.P.op("dve", lambda h: h.tensor_scalar(out=out, in0=in0, scalar1=s1, scalar2=None, op0=op0), reads, writes)
    else:
        C.P.op("dve", lambda h: h.tensor_scalar(out=out, in0=in0, scalar1=s1, scalar2=s2, op0=op0, op1=op1), reads, writes)


def STT(C, out, in0, scalar, in1, op0, op1, reads, writes):
    C.P.op("dve", lambda h: h.scalar_tensor_tensor(out=out, in0=in0, scalar=scalar, in1=in1, op0=op0, op1=op1), reads, writes)


def CP(C, eng, out, in_, reads, writes, scale=None):
    if eng == "act":
        if scale is None:
            C.P.op("act", lambda h: h.copy(out=out, in_=in_), reads, writes)
        else:
            C.P.op("act", lambda h: h.mul(out=out, in_=in_, mul=scale), reads, writes)
    else:
        if scale is None:
            C.P.op("dve", lambda h: h.tensor_copy(out=out, in_=in_), reads, writes)
        else:
            TS(C, out, in_, scale, None, ALU.mult, None, reads, writes)


def MS(C, ap, val, writes):
    C.P.op("dve", lambda h: h.memset(ap, val), (), writes)


def RED(C, out, in_, op, reads, writes):
    C.P.op("dve", lambda h: h.tensor_reduce(out=out, in_=in_, axis=AX.X, op=op), reads, writes)


def RCP(C, out, in_, reads, writes):
    C.P.op("dve", lambda h: h.reciprocal(out=out, in_=in_), reads, writes)


def SCAN(C, out, d0, d1, init, reads, writes):
    C.P.op("dve", lambda h: h.tensor_tensor_scan(out=out, data0=d0, data1=d1, initial=init, op0=ALU.mult, op1=ALU.add), reads, writes)


def DMA(C, out, in_, reads=(), writes=(), **kw):
    C.P.dma("sp", out, in_, reads, writes, **kw)


def rstd_ops(C, ss, ln, rstd, inv_n):
    ACT(C, ln[0], ss[0], AF.Ln, [ss[1]], [ln[1]], scale=inv_n, bias=EPS)
    ACT(C, rstd[0], ln[0], AF.Exp, [ln[1]], [rstd[1]], scale=-0.5)


def mm_group(C, out_ap, bank, pairs, reads):
    n = len(pairs)
    for k, (a, b) in enumerate(pairs):
        MM(C, out_ap, a, b, k == 0, k == n - 1, reads, [bank.b], signal=(k == n - 1))


def load_weight_bf16(C, dst_tile, dst_aps, src_aps, stage_tiles):
    for k, (dap, src) in enumerate(zip(dst_aps, src_aps)):
        stg = stage_tiles[k % len(stage_tiles)]
        shp = list(src.shape)
        sap = stg.t[:]
        if list(sap.shape) != shp:
            if len(shp) == 2:
                sap = stg.t[0:shp[0], 0:shp[1]]
            else:
                sap = stg.t[0:shp[0], 0:shp[1] * shp[2]].rearrange("p (a b) -> p a b", a=shp[1])
        DMA(C, sap, src, writes=[stg.b])
        CP(C, "act", dap, sap, [stg.b], [dst_tile.b])


def norm_tile(C, src_dram_ap, xt, junk, small, gain_bc, out_ap, out_buf):
    ss, ln, rstd = small
    DMA(C, xt[:], src_dram_ap, writes=[xt.b])
    ACT(C, junk[:], xt[:], AF.Square, [xt.b], [junk.b, ss.b], accum_out=ss[:])
    rstd_ops(C, (ss[:], ss.b), (ln[:], ln.b), (rstd[:], rstd.b), 1.0 / DM)
    STT(C, out_ap, xt[:], rstd[:], gain_bc[:], ALU.mult, ALU.mult, [xt.b, rstd.b, gain_bc.b], [out_buf])


def norm_transpose_tile(C, src_dram_ap, xt, junk, small, hn, gain_bc, hnT, col0):
    norm_tile(C, src_dram_ap, xt, junk, small, gain_bc, hn[:], hn.b)
    bank = next_bank(C)
    bv = bf16view(bank)
    for k in range(8):
        TR(C, bv[:, k * 128:(k + 1) * 128], hn[:, k * 128:(k + 1) * 128], C.idb[:], [hn.b, C.idb.b], [bank.b], signal=(k == 7))
    CP(C, "dve", hnT[:, :, col0:col0 + 128], bv.rearrange("p (k t) -> p k t", k=8), [bank.b], [hnT.b])


def norm_scratch(sb):
    xts = [sb(f"xt{i}", [128, DM], F32) for i in range(2)]
    junk = sb("junk", [128, DM], BF16)
    hns = [sb(f"hn{i}", [128, DM], BF16) for i in range(2)]
    smalls = [[sb(f"{n}{i}", [128, 1], F32) for n in ("ss", "ln", "rstd")] for i in range(2)]
    return xts, junk, hns, smalls


def phase_mem(C):
    nc, P, I = C.nc, C.P, C.I
    with contextlib.ExitStack() as ph:
        def sb(name, shape, dt):
            return Tile(ph.enter_context(nc.sbuf_tensor("M_" + name, shape, dt)), name)
        gain = sb("gain", [128, DM], F32)
        DMA(C, gain[:], I["mem_norm"].partition_broadcast(128), writes=[gain.b])
        xts, junk, hns, smalls = norm_scratch(sb)
        for i in range(C.NSEQ * MEM // 128):
            norm_transpose_tile(C, I["mem"][i * 128:(i + 1) * 128, :], xts[i % 2], junk, smalls[i % 2], hns[i % 2],
                                gain, C.memnT, i * 128)
        P.barrier()


def phase_A(C, l):
    nc, P, I, D_ = C.nc, C.P, C.I, C.D
    S, NSEQ, NG, NT = C.S, C.NSEQ, C.NG, C.NT
    hsrc = C.hcur
    with contextlib.ExitStack() as ph:
        def sb(name, shape, dt):
            return Tile(ph.enter_context(nc.sbuf_tensor(f"A{l}_" + name, shape, dt)), name)
        win = sb("win", [128, 8, O_END], BF16)
        HW_ = O_END // 2
        wst = [sb(f"wst{i}", [128, HW_], F32) for i in range(2)]
        load_weight_bf16(C, win, [win[:, k // 2, (k % 2) * HW_:(k % 2 + 1) * HW_] for k in range(16)],
                         [I["w_in"][l, (k // 2) * 128:(k // 2 + 1) * 128, (k % 2) * HW_:(k % 2 + 1) * HW_] for k in range(16)], wst)
        wup = sb("wup", [16, 256], BF16)
        load_weight_bf16(C, wup, [wup[:]], [I["w_alpha_up"][l]], wst)
        gain = sb("gain", [128, DM], F32)
        DMA(C, gain[:], I["mix_norm"][l].partition_broadcast(128), writes=[gain.b])
        nba = sb("nba", [64, 4], F32)
        nbf = sb("nbf", [8, 1], F32)
        DMA(C, nba[:], I["b_alpha"][l].rearrange("(h d) -> d h", d=64), writes=[nba.b], allow_slow_non_contiguous=True)
        DMA(C, nbf[:], I["b_forget"][l].rearrange("(h o) -> h o", o=1), writes=[nbf.b])
        CP(C, "act", nba[:], nba[:], [nba.b], [nba.b], scale=-1.0)
        CP(C, "act", nbf[:], nbf[:], [nbf.b], [nbf.b], scale=-1.0)
        ones8 = sb("ones8", [8, 512], F32)
        MS(C, ones8[:], 1.0, [ones8.b])
        xts, junk, hns, smalls = norm_scratch(sb)
        hnTs = [sb(f"hnT{i}", [128, 8, 512], BF16) for i in range(2)]
        stqs = [sb(f"stq{i}", [128, 8, 512], BF16) for i in range(1)] * 2
        stvs = [sb(f"stv{i}", [128, 4, 8, 65], BF16) for i in range(2)]
        for t_ in stvs:
            MS(C, t_[:], 1.0, [t_.b])
        stgv = [sb(f"stgv{i}", [128, 4, 512], BF16) for i in range(1)] * 2
        stgg = [sb(f"stgg{i}", [128, 4, 512], BF16) for i in range(1)] * 2
        gaT = sb("gaT", [16, 512], BF16)
        e1 = sb("e1", [64, 4, 512], F32)
        LgG = sb("LgG", [64, 4, 513], F32)
        Dg = sb("Dg", [64, 4, 512], F32)
        expb = sb("expb", [64, 4, 512], F32)
        expnb = e1
        qts = [sb(f"qgt{i}", [64, 4, 512], BF16) for i in range(2)]
        kts = [sb(f"kgt{i}", [64, 4, 512], BF16) for i in range(2)]
        ktoks = [sb(f"ktok{i}", [128, 4, 256], BF16) for i in range(2)]
        ebl = sb("ebl", [64, 4, NT], F32)
        ef = sb("ef", [8, 512], F32)
        lf = sb("lf", [8, 512], F32)
        Lf = sb("Lf", [8, 513], F32)
        nLf = sb("nLf", [8, 512], F32)
        P.barrier()

        gi = 0
        for b in range(NSEQ):
            MS(C, LgG[:, :, 0:1], 0.0, [LgG.b])
            MS(C, Lf[:, 0:1], 0.0, [Lf.b])
            for g in range(NG):
                hnT, stq, stv, sgv, sgg = hnTs[gi % 2], stqs[gi % 2], stvs[gi % 2], stgv[gi % 2], stgg[gi % 2]
                qt, kt, ktok = qts[gi % 2], kts[gi % 2], ktoks[gi % 2]
                gi += 1
                tg = b * S + g * 512
                gsl = slice(g * 512, (g + 1) * 512)
                for i in range(4):
                    t0 = tg + i * 128
                    norm_transpose_tile(C, hsrc[t0:t0 + 128, :], xts[i % 2], junk, smalls[i % 2], hns[i % 2], gain, hnT, i * 128)
                for c in range(8):
                    col = (O_FQ if c < 4 else O_FK) + (c % 4) * 128
                    bank = next_bank(C)
                    mm_group(C, bank[:], bank, [(win[:, k, col:col + 128], hnT[:, k, :]) for k in range(8)], [win.b, hnT.b])
                    CP(C, "act" if c % 2 == 0 else "dve", stq[:, c, :], bank[:], [bank.b], [stq.b], scale=(0.125 if c < 4 else None))
                DMA(C, D_["QT"][b, :, :, gsl].rearrange("c p t -> p c t"), stq[:, 0:4, :], reads=[stq.b])
                DMA(C, D_["KT"][b, :, :, gsl].rearrange("c p t -> p c t"), stq[:, 4:8, :], reads=[stq.b])
                bank = next_bank(C)
                mm_group(C, bank[0:8, :], bank, [(win[:, k, O_FF:O_FF + 8], hnT[:, k, :]) for k in range(8)], [win.b, hnT.b])
                ACT(C, ef[:], bank[0:8, :], AF.Exp, [bank.b, nbf.b], [ef.b], scale=-1.0, bias=nbf[:])
                ACT(C, lf[:], ef[:], AF.Ln, [ef.b], [lf.b], bias=1.0)
                SCAN(C, Lf[:, 1:513], ones8[:], lf[:], Lf[:, 0:1], [ones8.b, lf.b, Lf.b], [Lf.b])
                CP(C, "act", nLf[:], Lf[:, 1:513], [Lf.b], [nLf.b], scale=-1.0)
                DMA(C, D_["A"][b, :, 0, gsl], Lf[:, 1:513], reads=[Lf.b])
                DMA(C, D_["A"][b, :, 1, gsl], ones8[:], reads=[ones8.b])
                DMA(C, D_["B"][b, :, 0, gsl], ones8[:], reads=[ones8.b])
                DMA(C, D_["B"][b, :, 1, gsl], nLf[:], reads=[nLf.b])
                CP(C, "dve", Lf[:, 0:1], Lf[:, 512:513], [Lf.b], [Lf.b])
                bank = next_bank(C)
                mm_group(C, bank[0:16, :], bank, [(win[:, k, O_GA:O_GA + 16], hnT[:, k, :]) for k in range(8)], [win.b, hnT.b])
                CP(C, "act", gaT[:], bank[0:16, :], [bank.b], [gaT.b])
                for hh in range(4):
                    bank = next_bank(C)
                    mm_group(C, bank[0:64, :], bank, [(wup[:, hh * 64:(hh + 1) * 64], gaT[:])], [wup.b, gaT.b])
                    ACT(C, e1[:, hh, :], bank[0:64, :], AF.Exp, [bank.b, nba.b], [e1.b], scale=-1.0, bias=nba[:, hh:hh + 1])
                ACT(C, e1[:], e1[:], AF.Ln, [e1.b], [e1.b], bias=1.0)
                for hh in range(4):
                    SCAN(C, LgG[:, hh, 1:513], C.ones[0:64, :], e1[:, hh, :], LgG[:, hh, 0:1], [C.ones.b, e1.b, LgG.b], [LgG.b])
                TT(C, Dg[:].rearrange("p h (c t) -> p h c t", t=128),
                   LgG[:, :, 1:513].rearrange("p h (c t) -> p h c t", t=128),
                   LgG[:, :, 0:512].rearrange("p h (c t) -> p h c t", t=128)[:, :, :, 0:1].to_broadcast([64, 4, 4, 128]),
                   ALU.subtract, [LgG.b], [Dg.b])
                ACT(C, expb[:], Dg[:], AF.Exp, [Dg.b], [expb.b], scale=-1.0 / 16)
                ACT(C, expnb[:], Dg[:], AF.Exp, [Dg.b], [expnb.b], scale=1.0 / 16)
                CP(C, "dve", ebl[:, :, g * 4:(g + 1) * 4], expb[:].rearrange("p h (c t) -> p h c t", t=128)[:, :, :, 127], [expb.b], [ebl.b])
                CP(C, "dve", LgG[:, :, 0:1], LgG[:, :, 512:513], [LgG.b], [LgG.b])
                for hh in range(4):
                    bank = next_bank(C)
                    col = O_GQ + hh * 64
                    mm_group(C, bank[0:64, :], bank, [(win[:, k, col:col + 64], hnT[:, k, :]) for k in range(8)], [win.b, hnT.b])
                    STT(C, qt[:, hh, :], bank[0:64, :], 0.125, expb[:, hh, :], ALU.mult, ALU.mult, [bank.b, expb.b], [qt.b])
                    bank = next_bank(C)
                    col = O_GK + hh * 64
                    mm_group(C, bank[0:64, :], bank, [(win[:, k, col:col + 64], hnT[:, k, :]) for k in range(8)], [win.b, hnT.b])
                    TT(C, kt[:, hh, :], bank[0:64, :], expnb[:, hh, :], ALU.mult, [bank.b, expnb.b], [kt.b])
                DMA(C, D_["qgT"][b, :, :, gsl], qt[:], reads=[qt.b])
                DMA(C, D_["kgT"][b, :, :, gsl], kt[:], reads=[kt.b])
                bank = next_bank(C)
                bv = bf16view(bank)
                for c in range(4):
                    for hh in range(4):
                        o0 = c * 256 + hh * 64
                        TR(C, bv[:, o0:o0 + 64], kt[:, hh, c * 128:(c + 1) * 128], C.idb[0:64, 0:64], [kt.b, C.idb.b], [bank.b],
                           signal=(c == 3 and hh == 3))
                CP(C, "act", ktok[:], bv.rearrange("p (c f) -> p c f", c=4), [bank.b], [ktok.b])
                DMA(C, D_["ktok"][tg:tg + 512, :].rearrange("(c p) f -> p c f", p=128), ktok[:], reads=[ktok.b])
                for i in range(4):
                    lhs = [hnT[:, k, i * 128:(i + 1) * 128] for k in range(8)]
                    bank = next_bank(C)
                    mm_group(C, bank[:], bank, [(lhs[k], win[:, k, O_FV:O_FV + 512]) for k in range(8)], [win.b, hnT.b])
                    CP(C, "dve", stv[:, i, :, 0:64], bank[:].rearrange("p (h d) -> p h d", d=64), [bank.b], [stv.b])
                    bank = next_bank(C)
                    mm_group(C, bank[:], bank, [(lhs[k], win[:, k, O_GV:O_GV + 512]) for k in range(8)], [win.b, hnT.b])
                    CP(C, "act", sgv[:, i, :], bank[:], [bank.b], [sgv.b])
                    bank = next_bank(C)
                    mm_group(C, bank[:], bank, [(lhs[k], win[:, k, O_GG:O_GG + 512]) for k in range(8)], [win.b, hnT.b])
                    ACT(C, sgg[:, i, :], bank[:], AF.Silu, [bank.b], [sgg.b])
                DMA(C, D_["vaug"][tg:tg + 512, :].rearrange("(i p) f -> p i f", p=128), stv[:].rearrange("p i h d -> p i (h d)"), reads=[stv.b])
                DMA(C, D_["gv"][tg:tg + 512, :].rearrange("(i p) f -> p i f", p=128), sgv[:], reads=[sgv.b])
                DMA(C, D_["gg"][tg:tg + 512, :].rearrange("(i p) f -> p i f", p=128), sgg[:], reads=[sgg.b])
            DMA(C, D_["ebl"][b], ebl[:], reads=[ebl.b])
        P.barrier()


def phase_B1(C, l):
    nc, P, I, D_ = C.nc, C.P, C.I, C.D
    S, NSEQ, NT = C.S, C.NSEQ, C.NT
    with contextlib.ExitStack() as ph:
        def sb(name, shape, dt):
            return Tile(ph.enter_context(nc.sbuf_tensor(f"B1{l}_" + name, shape, dt)), name)
        QT = sb("QT", [128, 4, S], BF16)
        KT = sb("KT", [128, 4, S], BF16)
        Va = sb("Va", [128, NT, 520], BF16)
        Ars = [sb(f"Ar{i}", [128, S], F32) for i in range(2)]
        Brs = [sb(f"Br{i}", [128, S], F32) for i in range(2)]
        for t_ in Ars + Brs:
            MS(C, t_[:], 0.0, [t_.b])
        PTs = [sb(f"PT{i}", [128, NT, 128], BF16) for i in range(2)]
        fo = sb("fo", [128, NT, 512], F32)
        gain = sb("gain", [128, 512], F32)
        DMA(C, gain[:], I["fox_out_gain"][l].partition_broadcast(128), writes=[gain.b])
        recs = [sb(f"rec{i}", [128, 1], F32) for i in range(2)]
        tmp = sb("tmp", [128, 512], F32)
        ssq = sb("ssq", [128, 8], F32)
        lnq = sb("lnq", [128, 8], F32)
        rsq = sb("rsq", [128, 8], F32)
        ybs = [sb(f"yb{i}", [128, 512], BF16) for i in range(2)]
        P.barrier()
        sbanks = C.banks[0:5]
        abanks = C.banks[5:8]
        si = ai = hi = 0
        for b in range(NSEQ):
            DMA(C, QT[:], D_["QT"][b].rearrange("c p t -> p c t"), writes=[QT.b])
            DMA(C, KT[:], D_["KT"][b].rearrange("c p t -> p c t"), writes=[KT.b])
            DMA(C, Va[:], D_["vaug"][b * S:(b + 1) * S, :].rearrange("(i p) f -> p i f", p=128), writes=[Va.b])
            for hh in range(8):
                c = hh // 2
                pr = (hh % 2) * 64
                Ar, Br = Ars[hi % 2], Brs[hi % 2]
                hi += 1
                assert hi % 2 == (hh + 1) % 2
                DMA(C, Ar[pr:pr + 2, :], D_["A"][b, hh], writes=[Ar.b])
                DMA(C, Br[pr:pr + 2, :], D_["B"][b, hh], writes=[Br.b])
                pend = None
                for j in range(NT + 1):
                    if j < NT:
                        PT = PTs[j % 2]
                        for i0 in range(0, j + 1, 4):
                            nb = min(4, j + 1 - i0)
                            bank = sbanks[si % 5]
                            si += 1
                            for i in range(i0, i0 + nb):
                                o = bank[:, (i - i0) * 128:(i - i0 + 1) * 128]
                                last = (i == i0 + nb - 1)
                                MM(C, o, KT[pr:pr + 64, c, i * 128:(i + 1) * 128], QT[pr:pr + 64, c, j * 128:(j + 1) * 128], True, False,
                                   [KT.b, QT.b], [bank.b], signal=False)
                                MM(C, o, Ar[pr:pr + 64, i * 128:(i + 1) * 128], Br[pr:pr + 64, j * 128:(j + 1) * 128], False, (i != j),
                                   [Ar.b, Br.b], [bank.b], signal=(last and i != j))
                                if i == j:
                                    MM(C, o, C.idb[:], C.maskadd[:], False, True, [C.idb.b, C.maskadd.b], [bank.b], signal=True)
                            ACT(C, PT[:, i0:i0 + nb, :], bank[:, 0:nb * 128].rearrange("p (i t) -> p i t", t=128), AF.Exp, [bank.b], [PT.b])
                    if pend is not None:
                        jj, PTp = pend
                        ab = abanks[ai % 3]
                        rec = recs[ai % 2]
                        ai += 1
                        for i in range(jj + 1):
                            MM(C, ab[:, 0:65], PTp[:, i, :], Va[:, i, hh * 65:(hh + 1) * 65], i == 0, i == jj, [PTp.b, Va.b], [ab.b],
                               signal=(i == jj))
                        RCP(C, rec[:], ab[:, 64:65], [ab.b], [rec.b])
                        TS(C, fo[:, jj, hh * 64:(hh + 1) * 64], ab[:, 0:64], rec[:], None, ALU.mult, None, [ab.b, rec.b], [fo.b])
                    pend = (j, PTs[j % 2]) if j < NT else None
            for j in range(NT):
                yb = ybs[j % 2]
                TT(C, tmp[:], fo[:, j, :], fo[:, j, :], ALU.mult, [fo.b], [tmp.b])
                RED(C, ssq[:], tmp[:].rearrange("p (h d) -> p h d", d=64), ALU.add, [tmp.b], [ssq.b])
                rstd_ops(C, (ssq[:], ssq.b), (lnq[:], lnq.b), (rsq[:], rsq.b), 1.0 / 64)
                TT(C, tmp[:].rearrange("p (h d) -> p h d", d=64), fo[:, j, :].rearrange("p (h d) -> p h d", d=64),
                   rsq[:].unsqueeze(2).to_broadcast([128, 8, 64]), ALU.mult, [fo.b, rsq.b], [tmp.b])
                TT(C, yb[:], tmp[:], gain[:], ALU.mult, [tmp.b, gain.b], [yb.b])
                r0 = b * S + j * 128
                DMA(C, D_["ymix"][r0:r0 + 128, 0:512], yb[:], reads=[yb.b])
        P.barrier()


def phase_B2(C, l):
    nc, P, I, D_ = C.nc, C.P, C.I, C.D
    S, NSEQ, NT = C.S, C.NSEQ, C.NT
    with contextlib.ExitStack() as ph:
        def sb(name, shape, dt):
            return Tile(ph.enter_context(nc.sbuf_tensor(f"B2{l}_" + name, shape, dt)), name)
        qg = sb("qg", [64, 4, S], BF16)
        kg = sb("kg", [64, 4, S], BF16)
        ktok = sb("ktok", [128, NT, 256], BF16)
        gv = sb("gv", [128, NT, 512], BF16)
        gg = sb("gg", [128, NT, 512], BF16)
        ebl = sb("ebl", [64, 4, NT], F32)
        Sf = sb("Sf", [64, 4, 128], F32)
        Sb_ = sb("Sb", [64, 4, 128], BF16)
        tS = sb("tS", [64, 4, 128], F32)
        atts = [sb(f"att{i}", [128, 4, 128], BF16) for i in range(2)]
        gain = sb("gain", [128, 512], F32)
        DMA(C, gain[:], I["gla_out_gain"][l].partition_broadcast(128), writes=[gain.b])
        sq = sb("sq", [128, 512], F32)
        ssq = sb("ssq", [128, 4], F32)
        lnq = sb("lnq", [128, 4], F32)
        rsq = sb("rsq", [128, 4], F32)
        t1 = sb("t1", [128, 512], F32)
        ybs = [sb(f"yb{i}", [128, 512], BF16) for i in range(2)]
        for b in range(NSEQ):
            DMA(C, qg[:], D_["qgT"][b], writes=[qg.b])
            DMA(C, kg[:], D_["kgT"][b], writes=[kg.b])
            DMA(C, ktok[:], D_["ktok"][b * S:(b + 1) * S, :].rearrange("(i p) f -> p i f", p=128), writes=[ktok.b])
            DMA(C, gv[:], D_["gv"][b * S:(b + 1) * S, :].rearrange("(i p) f -> p i f", p=128), writes=[gv.b])
            DMA(C, gg[:], D_["gg"][b * S:(b + 1) * S, :].rearrange("(i p) f -> p i f", p=128), writes=[gg.b])
            DMA(C, ebl[:], D_["ebl"][b], writes=[ebl.b])
            MS(C, Sf[:], 0.0, [Sf.b])
            MS(C, Sb_[:], 0.0, [Sb_.b])
            for c in range(NT):
                att = atts[c % 2]
                yb = ybs[c % 2]
                cs = slice(c * 128, (c + 1) * 128)
                bk_a = next_bank(C)
                for hh in range(4):
                    MM(C, bk_a[:, hh * 128:(hh + 1) * 128], kg[:, hh, cs], qg[:, hh, cs], True, True, [kg.b, qg.b], [bk_a.b], signal=(hh == 3))
                TT(C, att[:], bk_a[:].rearrange("p (h t) -> p h t", t=128), C.mask01[:].unsqueeze(1).to_broadcast([128, 4, 128]), ALU.mult,
                   [bk_a.b, C.mask01.b], [att.b])
                bk_o = next_bank(C)
                for hh in range(4):
                    o = bk_o[:, hh * 128:(hh + 1) * 128]
                    MM(C, o, qg[:, hh, cs], Sb_[:, hh, :], True, False, [qg.b, Sb_.b], [bk_o.b], signal=False)
                    MM(C, o, att[:, hh, :], gv[:, c, hh * 128:(hh + 1) * 128], False, True, [att.b, gv.b], [bk_o.b], signal=(hh == 3))
                bk_s = next_bank(C)
                for hh in range(4):
                    MM(C, bk_s[0:64, hh * 128:(hh + 1) * 128], ktok[:, c, hh * 64:(hh + 1) * 64], gv[:, c, hh * 128:(hh + 1) * 128], True, True,
                       [ktok.b, gv.b], [bk_s.b], signal=(hh == 3))
                TT(C, tS[:].rearrange("p h v -> p (h v)"), bk_s[0:64, :], Sf[:].rearrange("p h v -> p (h v)"), ALU.add, [bk_s.b, Sf.b], [tS.b])
                TT(C, Sf[:], tS[:], ebl[:, :, c:c + 1].to_broadcast([64, 4, 128]), ALU.mult, [tS.b, ebl.b], [Sf.b])
                CP(C, "act", Sb_[:], Sf[:], [Sf.b], [Sb_.b])
                ACT(C, sq[:], bk_o[:], AF.Square, [bk_o.b], [sq.b])
                RED(C, ssq[:], sq[:].rearrange("p (h d) -> p h d", d=128), ALU.add, [sq.b], [ssq.b])
                rstd_ops(C, (ssq[:], ssq.b), (lnq[:], lnq.b), (rsq[:], rsq.b), 1.0 / 128)
                TT(C, t1[:].rearrange("p (h d) -> p h d", d=128), bk_o[:].rearrange("p (h d) -> p h d", d=128),
                   rsq[:].unsqueeze(2).to_broadcast([128, 4, 128]), ALU.mult, [bk_o.b, rsq.b], [t1.b])
                TT(C, t1[:], t1[:], gain[:], ALU.mult, [t1.b, gain.b], [t1.b])
                TT(C, yb[:], t1[:], gg[:, c, :], ALU.mult, [t1.b, gg.b], [yb.b])
                r0 = b * S + c * 128
                DMA(C, D_["ymix"][r0:r0 + 128, 512:1024], yb[:], reads=[yb.b])
        P.barrier()


def proj_residual(C, l, tagname, ysrc_fn, kchunks, w_dram, hsrc, hdst, ntiles, final_gain=None):
    nc, P = C.nc, C.P
    with contextlib.ExitStack() as ph:
        def sb(name, shape, dt):
            return Tile(ph.enter_context(nc.sbuf_tensor(f"{tagname}{l}_" + name, shape, dt)), name)
        wo = sb("wo", [128, kchunks, DM], BF16)
        wst = [sb(f"wst{i}", [128, DM], F32) for i in range(2)]
        load_weight_bf16(C, wo, [wo[:, k, :] for k in range(kchunks)], [w_dram[k * 128:(k + 1) * 128, :] for k in range(kchunks)], wst)
        yts = [sb(f"yt{i}", [128, kchunks * 128], BF16) for i in range(2)]
        yTs = [sb(f"yT{i}", [128, kchunks, 128], BF16) for i in range(2)]
        hts = [sb(f"ht{i}", [128, DM], F32) for i in range(2)]
        for i in range(ntiles):
            yt, yT, ht = yts[i % 2], yTs[i % 2], hts[i % 2]
            DMA(C, yt[:], ysrc_fn(i), writes=[yt.b])
            DMA(C, ht[:], hsrc[i * 128:(i + 1) * 128, :], writes=[ht.b])
            bank = next_bank(C)
            bv = bf16view(bank)
            for k in range(kchunks):
                TR(C, bv[:, k * 128:(k + 1) * 128], yt[:, k * 128:(k + 1) * 128], C.idb[:], [yt.b, C.idb.b], [bank.b], signal=(k == kchunks - 1))
            CP(C, "act", yT[:], bv[:, 0:kchunks * 128].rearrange("p (k t) -> p k t", k=kchunks), [bank.b], [yT.b])
            for n in range(2):
                bank = next_bank(C)
                mm_group(C, bank[:], bank, [(yT[:, k, :], wo[:, k, n * 512:(n + 1) * 512]) for k in range(kchunks)], [yT.b, wo.b])
                TT(C, ht[:, n * 512:(n + 1) * 512], bank[:], ht[:, n * 512:(n + 1) * 512], ALU.add, [bank.b, ht.b], [ht.b])
            DMA(C, hdst[i * 128:(i + 1) * 128, :], ht[:], reads=[ht.b])
        P.barrier()


def hnext(C):
    return C.D["h2"] if C.hcur is C.D["h"] else C.D["h"]


def phase_B3(C, l):
    dst = hnext(C)
    proj_residual(C, l, "B3", lambda i: C.D["ymix"][i * 128:(i + 1) * 128, :], 8, C.I["w_out"][l], C.hcur, dst, C.T // 128)
    C.hcur = dst


def phase_C(C, l):
    nc, P, I, D_ = C.nc, C.P, C.I, C.D
    S, NSEQ, NG = C.S, C.NSEQ, C.NG
    XS = 128 ** -0.5
    with contextlib.ExitStack() as ph:
        def sb(name, shape, dt):
            return Tile(ph.enter_context(nc.sbuf_tensor(f"C{l}_" + name, shape, dt)), name)
        wst = [sb(f"wst{i}", [128, 4, 512], F32) for i in range(2)]
        wq = sb("wq", [128, 8, 512], BF16)
        wk = sb("wk", [128, 8, 512], BF16)
        wv = sb("wv", [128, 8, 512], BF16)
        for wt, name in ((wk, "w_xk"), (wv, "w_xv"), (wq, "w_xq")):
            src = I[name][l].rearrange("(k p) f -> p k f", p=128)
            load_weight_bf16(C, wt, [wt[:, 0:4, :], wt[:, 4:8, :]], [src[:, 0:4, :], src[:, 4:8, :]], wst)
        gain = sb("gain", [128, DM], F32)
        DMA(C, gain[:], I["cross_norm"][l].partition_broadcast(128), writes=[gain.b])
        KmT = sb("KmT", [128, NSEQ, 4, MEM], BF16)
        Vm = sb("Vm", [128, NSEQ, 2, 4, 129], BF16)
        MS(C, Vm[:], 1.0, [Vm.b])
        for b in range(NSEQ):
            for hh in range(4):
                bank = next_bank(C)
                mm_group(C, bank[:, 0:MEM], bank, [(wk[:, k, hh * 128:(hh + 1) * 128], C.memnT[:, k, b * MEM:(b + 1) * MEM]) for k in range(8)],
                         [wk.b, C.memnT.b])
                CP(C, "act", KmT[:, b, hh, :], bank[:, 0:MEM], [bank.b], [KmT.b], scale=XS)
            for mb in range(2):
                bank = next_bank(C)
                mm_group(C, bank[:], bank, [(C.memnT[:, k, b * MEM + mb * 128:b * MEM + (mb + 1) * 128], wv[:, k, :]) for k in range(8)],
                         [wv.b, C.memnT.b])
                CP(C, "dve", Vm[:, b, mb, :, 0:128], bank[:].rearrange("p (h d) -> p h d", d=128), [bank.b], [Vm.b])
        xts, junk, hns, smalls = norm_scratch(sb)
        hnTs = [sb(f"hnT{i}", [128, 8, 512], BF16) for i in range(2)]
        qTs = [sb(f"qT{i}", [128, 4, 512], BF16) for i in range(2)]
        PTs = [sb(f"PT{i}", [128, 2, 512], BF16) for i in range(2)]
        obs = [sb(f"ob{i}", [128, 4, 512], BF16) for i in range(2)]
        recs = [sb(f"rec{i}", [128, 1], F32) for i in range(2)]
        P.barrier()
        cstop = C.cfg.get("cstop", 9)
        gi = pi = ri = 0
        for b in range(NSEQ):
            if cstop <= 1:
                break
            for g in range(NG):
                hnT, qT, ob = hnTs[gi % 2], qTs[gi % 2], obs[gi % 2]
                gi += 1
                tg = b * S + g * 512
                for i in range(4):
                    t0 = tg + i * 128
                    norm_transpose_tile(C, C.hcur[t0:t0 + 128, :], xts[i % 2], junk, smalls[i % 2], hns[i % 2], gain, hnT, i * 128)
                for hh in range(4):
                    bank = next_bank(C)
                    mm_group(C, bank[:], bank, [(wq[:, k, hh * 128:(hh + 1) * 128], hnT[:, k, :]) for k in range(8)], [wq.b, hnT.b])
                    CP(C, "act" if hh % 2 == 0 else "dve", qT[:, hh, :], bank[:], [bank.b], [qT.b])
                for hh in range(4):
                    if cstop <= 2:
                        break
                    PT = PTs[pi % 2]
                    pi += 1
                    for mb in range(2):
                        bank = next_bank(C)
                        MM(C, bank[:], KmT[:, b, hh, mb * 128:(mb + 1) * 128], qT[:, hh, :], True, True, [KmT.b, qT.b], [bank.b])
                        ACT(C, PT[:, mb, :], bank[:], AF.Exp, [bank.b], [PT.b])
                    for half in range(2):
                        if cstop <= 3:
                            break
                        bank = next_bank(C)
                        for ii in range(2):
                            i = half * 2 + ii
                            o = bank[:, ii * 256:ii * 256 + 129]
                            for mb in range(2):
                                MM(C, o, PT[:, mb, i * 128:(i + 1) * 128], Vm[:, b, mb, hh, :], mb == 0, mb == 1, [PT.b, Vm.b], [bank.b],
                                   signal=(mb == 1 and ii == 1))
                        for ii in range(2):
                            i = half * 2 + ii
                            rec = recs[ri % 2]
                            ri += 1
                            RCP(C, rec[:], bank[:, ii * 256 + 128:ii * 256 + 129], [bank.b], [rec.b])
                            TS(C, ob[:, i, hh * 128:(hh + 1) * 128], bank[:, ii * 256:ii * 256 + 128], rec[:], None, ALU.mult, None,
                               [bank.b, rec.b], [ob.b])
                DMA(C, D_["ymix"][tg:tg + 512, 0:512].rearrange("(i p) f -> p i f", p=128), ob[:], reads=[ob.b])
        P.barrier()
    if C.cfg.get("cstop", 9) <= 4:
        return
    dst = hnext(C)
    proj_residual(C, l, "C3", lambda i: D_["ymix"][i * 128:(i + 1) * 128, 0:512], 4, I["w_xo"][l], C.hcur, dst, C.T // 128)
    C.hcur = dst


BIG = 10000.0


def phase_D(C, l, last):
    nc, P, I, D_ = C.nc, C.P, C.I, C.D
    S, NSEQ, NG, NT = C.S, C.NSEQ, C.NG, C.NT
    if C.cfg.get("nofinal"):
        last = False
    with contextlib.ExitStack() as ph:
        def sb(name, shape, dt):
            return Tile(ph.enter_context(nc.sbuf_tensor(f"D{l}_" + name, shape, dt)), name)
        gain = sb("gain", [128, DM], F32)
        DMA(C, gain[:], I["moe_norm"][l].partition_broadcast(128), writes=[gain.b])
        Wr = sb("Wr", [128, 8, 20], F32)
        DMA(C, Wr[:, :, 0:4], I["w_rg"][l].rearrange("(k p) f -> p k f", p=128), writes=[Wr.b], allow_slow_non_contiguous=True)
        DMA(C, Wr[:, :, 4:20], I["w_re"][l].rearrange("(k p) f -> p k f", p=128), writes=[Wr.b], allow_slow_non_contiguous=True)
        Wrh = sb("Wrh", [128, 8, 20], BF16)
        Wrl = sb("Wrl", [128, 8, 20], BF16)
        CP(C, "dve", Wrh[:], Wr[:], [Wr.b], [Wrh.b])
        TT(C, Wrl[:], Wr[:], Wrh[:], ALU.subtract, [Wr.b, Wrh.b], [Wrl.b])
        brb = sb("brb", [128, 20], F32)
        DMA(C, brb[:, 0:4], I["b_rg"][l].partition_broadcast(128), writes=[brb.b])
        DMA(C, brb[:, 4:20], I["b_re"][l].partition_broadcast(128), writes=[brb.b])
        hnT = sb("hnT", [128, 8, S], BF16)
        acc = sb("acc", [128, NT, DM], F32)
        lg = sb("lg", [128, NT, 20], F32)
        G = sb("G", [128, NT, 16], F32)
        if last:
            fgain = sb("fgain", [128, DM], F32)
            DMA(C, fgain[:], I["final_norm"].partition_broadcast(128), writes=[fgain.b])
        P.barrier()
        hdst = hnext(C)
        for b in range(NSEQ):
            with contextlib.ExitStack() as ph2:
                def sb2(name, shape, dt):
                    return Tile(ph2.enter_context(nc.sbuf_tensor(f"D{l}r{b}_" + name, shape, dt)), name)
                xts = [sb2(f"xt{i}", [128, DM], F32) for i in range(2)]
                junk = sb2("junk", [128, DM], BF16)
                hn32s = [sb2(f"hn32{i}", [128, DM], F32) for i in range(2)]
                his = [sb2(f"hi{i}", [128, DM], BF16) for i in range(2)]
                los = [sb2(f"lo{i}", [128, DM], BF16) for i in range(2)]
                loTs = [sb2(f"loT{i}", [128, 8, 128], BF16) for i in range(2)]
                smalls = [[sb2(f"{n}{i}", [128, 1], F32) for n in ("ss", "ln", "rstd")] for i in range(2)]
                sm = {n: sb2(n, [128, NT], F32) for n in ("gmax", "sg", "pg", "m1", "m2", "den", "coef")}
                g4 = {n: sb2(n, [128, NT, 4], F32) for n in ("gm", "eg", "pen")}
                e16 = {n: sb2(n, [128, NT, 16], F32) for n in ("lem", "isf", "le2", "sel", "w")}
                P.barrier()
                for i in range(NT):
                    t0 = b * S + i * 128
                    xt, hn32, hi_, lo_, loT = xts[i % 2], hn32s[i % 2], his[i % 2], los[i % 2], loTs[i % 2]
                    norm_tile(C, C.hcur[t0:t0 + 128, :], xt, junk, smalls[i % 2], gain, hn32[:], hn32.b)
                    CP(C, "act", hi_[:], hn32[:], [hn32.b], [hi_.b])
                    TT(C, lo_[:], hn32[:], hi_[:], ALU.subtract, [hn32.b, hi_.b], [lo_.b])
                    tsl = slice(i * 128, (i + 1) * 128)
                    bank = next_bank(C)
                    bv = bf16view(bank)
                    for k in range(8):
                        TR(C, bv[:, k * 128:(k + 1) * 128], hi_[:, k * 128:(k + 1) * 128], C.idb[:], [hi_.b, C.idb.b], [bank.b], signal=(k == 7))
                    CP(C, "act", hnT[:, :, tsl], bv.rearrange("p (k t) -> p k t", k=8), [bank.b], [hnT.b])
                    bank = next_bank(C)
                    bv = bf16view(bank)
                    for k in range(8):
                        TR(C, bv[:, k * 128:(k + 1) * 128], lo_[:, k * 128:(k + 1) * 128], C.idb[:], [lo_.b, C.idb.b], [bank.b], signal=(k == 7))
                    CP(C, "dve", loT[:], bv.rearrange("p (k t) -> p k t", k=8), [bank.b], [loT.b])
                    bank = next_bank(C)
                    pairs = []
                    for k in range(8):
                        pairs += [(hnT[:, k, tsl], Wrh[:, k, :]), (loT[:, k, :], Wrh[:, k, :]), (hnT[:, k, tsl], Wrl[:, k, :])]
                    mm_group(C, bank[:, 0:20], bank, pairs, [hnT.b, loT.b, Wrh.b, Wrl.b])
                    TT(C, lg[:, i, :], bank[:, 0:20], brb[:], ALU.add, [bank.b, brb.b], [lg.b])
                gl = lg[:, :, 0:4]
                el = lg[:, :, 4:20]
                bc4 = lambda t_: t_[:].unsqueeze(2).to_broadcast([128, NT, 4])
                bc16 = lambda t_: t_[:].unsqueeze(2).to_broadcast([128, NT, 16])
                RED(C, sm["gmax"][:], gl, ALU.max, [lg.b], [sm["gmax"].b])
                TT(C, g4["gm"][:], gl, bc4(sm["gmax"]), ALU.is_ge, [lg.b, sm["gmax"].b], [g4["gm"].b])
                TT(C, g4["eg"][:], gl, bc4(sm["gmax"]), ALU.subtract, [lg.b, sm["gmax"].b], [g4["eg"].b])
                ACT(C, g4["eg"][:], g4["eg"][:], AF.Exp, [g4["eg"].b], [g4["eg"].b])
                RED(C, sm["sg"][:], g4["eg"][:], ALU.add, [g4["eg"].b], [sm["sg"].b])
                RCP(C, sm["pg"][:], sm["sg"][:], [sm["sg"].b], [sm["pg"].b])
                TS(C, g4["pen"][:], g4["gm"][:], BIG, -BIG, ALU.mult, ALU.add, [g4["gm"].b], [g4["pen"].b])
                for gq_ in range(4):
                    TT(C, e16["lem"][:, :, gq_ * 4:(gq_ + 1) * 4], lg[:, :, 4 + gq_ * 4:8 + gq_ * 4],
                       g4["pen"][:, :, gq_:gq_ + 1].to_broadcast([128, NT, 4]), ALU.add, [lg.b, g4["pen"].b], [e16["lem"].b])
                RED(C, sm["m1"][:], e16["lem"][:], ALU.max, [e16["lem"].b], [sm["m1"].b])
                TT(C, e16["isf"][:], e16["lem"][:], bc16(sm["m1"]), ALU.is_ge, [e16["lem"].b, sm["m1"].b], [e16["isf"].b])
                STT(C, e16["le2"][:], e16["isf"][:], -BIG, e16["lem"][:], ALU.mult, ALU.add, [e16["isf"].b, e16["lem"].b], [e16["le2"].b])
                RED(C, sm["m2"][:], e16["le2"][:], ALU.max, [e16["le2"].b], [sm["m2"].b])
                TT(C, e16["sel"][:], e16["lem"][:], bc16(sm["m2"]), ALU.is_ge, [e16["lem"].b, sm["m2"].b], [e16["sel"].b])
                TT(C, e16["w"][:], e16["lem"][:], bc16(sm["m1"]), ALU.subtract, [e16["lem"].b, sm["m1"].b], [e16["w"].b])
                ACT(C, e16["w"][:], e16["w"][:], AF.Exp, [e16["w"].b], [e16["w"].b])
                TT(C, e16["w"][:], e16["w"][:], e16["sel"][:], ALU.mult, [e16["w"].b, e16["sel"].b], [e16["w"].b])
                RED(C, sm["den"][:], e16["w"][:], ALU.add, [e16["w"].b], [sm["den"].b])
                RCP(C, sm["den"][:], sm["den"][:], [sm["den"].b], [sm["den"].b])
                TT(C, sm["coef"][:], sm["den"][:], sm["pg"][:], ALU.mult, [sm["den"].b, sm["pg"].b], [sm["coef"].b])
                TT(C, G[:], e16["w"][:], bc16(sm["coef"]), ALU.mult, [e16["w"].b, sm["coef"].b], [G.b])
                P.barrier()
            if C.cfg.get("dstop", 9) <= 1:
                continue
            with contextlib.ExitStack() as ph3:
                def sb3(name, shape, dt):
                    return Tile(ph3.enter_context(nc.sbuf_tensor(f"D{l}e{b}_" + name, shape, dt)), name)
                wst = [sb3(f"wst{i}", [128, 2048], F32) for i in range(2)]
                Wgs = [sb3(f"Wg{i}", [128, 8, DEXP], BF16) for i in range(2)]
                Wus = [sb3(f"Wu{i}", [128, 8, DEXP], BF16) for i in range(2)]
                Wds = [sb3(f"Wd{i}", [128, 4, DM], BF16) for i in range(2)]
                uTs = [sb3(f"uT{i}", [128, 4, 512], BF16) for i in range(2)]
                sgs = [sb3(f"sg{i}", [128, 512], BF16) for i in range(2)]
                P.barrier()
                ui = 0
                for e in range(NEXP):
                    Wg, Wu, Wd = Wgs[e % 2], Wus[e % 2], Wds[e % 2]
                    sg_ = I["w_eg"][l, e].rearrange("(k p) f -> p k f", p=128)
                    su_ = I["w_eu"][l, e].rearrange("(k p) f -> p k f", p=128)
                    sd_ = I["w_ed"][l, e].rearrange("(k p) f -> p k f", p=128)
                    load_weight_bf16(C, Wg, [Wg[:, 0:4, :], Wg[:, 4:8, :]], [sg_[:, 0:4, :], sg_[:, 4:8, :]], wst)
                    load_weight_bf16(C, Wu, [Wu[:, 0:4, :], Wu[:, 4:8, :]], [su_[:, 0:4, :], su_[:, 4:8, :]], wst)
                    load_weight_bf16(C, Wd, [Wd[:, 0:2, :], Wd[:, 2:4, :]], [sd_[:, 0:2, :], sd_[:, 2:4, :]], wst)
                    for g in range(NG):
                        uT = uTs[ui % 2]
                        ui += 1
                        xs = hnT[:, :, g * 512:(g + 1) * 512]
                        for fc in range(4):
                            sg = sgs[fc % 2]
                            bg = next_bank(C)
                            mm_group(C, bg[:], bg, [(Wg[:, k, fc * 128:(fc + 1) * 128], hnT[:, k, g * 512:(g + 1) * 512]) for k in range(8)],
                                     [Wg.b, hnT.b])
                            ACT(C, sg[:], bg[:], AF.Silu, [bg.b], [sg.b])
                            bu = next_bank(C)
                            mm_group(C, bu[:], bu, [(Wu[:, k, fc * 128:(fc + 1) * 128], hnT[:, k, g * 512:(g + 1) * 512]) for k in range(8)],
                                     [Wu.b, hnT.b])
                            TT(C, uT[:, fc, :], bu[:], sg[:], ALU.mult, [bu.b, sg.b], [uT.b])
                        for i in range(4):
                            ti = g * 4 + i
                            for n in range(2):
                                by = next_bank(C)
                                mm_group(C, by[:], by, [(uT[:, fc, i * 128:(i + 1) * 128], Wd[:, fc, n * 512:(n + 1) * 512]) for fc in range(4)],
                                         [uT.b, Wd.b])
                                a = acc[:, ti, n * 512:(n + 1) * 512]
                                if e == 0:
                                    TS(C, a, by[:], G[:, ti, e:e + 1], None, ALU.mult, None, [by.b, G.b], [acc.b])
                                else:
                                    STT(C, a, by[:], G[:, ti, e:e + 1], a, ALU.mult, ALU.add, [by.b, G.b, acc.b], [acc.b])
                P.barrier()
            if C.cfg.get("dstop", 9) <= 2:
                continue
            with contextlib.ExitStack() as ph4:
                def sb4(name, shape, dt):
                    return Tile(ph4.enter_context(nc.sbuf_tensor(f"D{l}f{b}_" + name, shape, dt)), name)
                hts = [sb4(f"ht{i}", [128, DM], F32) for i in range(2)]
                ots = [sb4(f"ot{i}", [128, DM], F32) for i in range(2)]
                junk = sb4("junk", [128, DM], BF16)
                smalls = [[sb4(f"{n}{i}", [128, 1], F32) for n in ("ss", "ln", "rstd")] for i in range(2)]
                P.barrier()
                for i in range(NT):
                    t0 = b * S + i * 128
                    ht = hts[i % 2]
                    ss, ln, rstd = smalls[i % 2]
                    DMA(C, ht[:], C.hcur[t0:t0 + 128, :], writes=[ht.b])
                    TT(C, ht[:], ht[:], acc[:, i, :], ALU.add, [ht.b, acc.b], [ht.b])
                    if not last:
                        DMA(C, hdst[t0:t0 + 128, :], ht[:], reads=[ht.b])
                    else:
                        ACT(C, junk[:], ht[:], AF.Square, [ht.b], [junk.b, ss.b], accum_out=ss[:])
                        rstd_ops(C, (ss[:], ss.b), (ln[:], ln.b), (rstd[:], rstd.b), 1.0 / DM)
                        ot = ots[i % 2]
                        STT(C, ot[:], ht[:], rstd[:], fgain[:], ALU.mult, ALU.mult, [ht.b, rstd.b, fgain.b], [ot.b])
                        odst = hdst if C.cfg.get("out_to_h") else C.out
                        DMA(C, odst[t0:t0 + 128, :], ot[:], reads=[ot.b])
                P.barrier()
        P.barrier()
        C.hcur = hdst


def consts():
    s = np.arange(128)[:, None]
    t = np.arange(128)[None, :]
    return {
        "c_ident": np.eye(128, dtype=np.float32),
        "c_maskadd": np.where(s <= t, 0.0, NEG).astype(np.float32),
        "c_mask01": (s <= t).astype(np.float32),
    }


_RENAME = {"w_rg": "w_router_group", "b_rg": "b_router_group", "w_re": "w_router_expert", "b_re": "b_router_expert",
           "w_eg": "w_expert_gate", "w_eu": "w_expert_up", "w_ed": "w_expert_down"}
N_CORES = 8


def kernel(**inputs):
    x = np.asarray(inputs["x"], dtype=np.float32)
    mem = np.asarray(inputs["mem"], dtype=np.float32)
    B, S, _ = x.shape
    L = int(np.asarray(inputs["w_in"]).shape[0])
    NSEQ = B // N_CORES
    nc, C = build(dict(S=S, NSEQ=NSEQ, L=L, phases="0ABCD", debug=False))
    shared = {k: np.ascontiguousarray(np.asarray(v, dtype=np.float32)) for k, v in inputs.items() if k not in ("x", "mem")}
    shared.update(consts())
    in_maps = []
    for c in range(N_CORES):
        m = dict(shared)
        m["x"] = np.ascontiguousarray(x[c * NSEQ:(c + 1) * NSEQ].reshape(NSEQ * S, DM))
        m["mem"] = np.ascontiguousarray(mem[c * NSEQ:(c + 1) * NSEQ].reshape(NSEQ * MEM, DM))
        in_maps.append(m)
    res = run_bass_kernel_spmd(nc, in_maps, core_ids=list(range(N_CORES)))
    outs = [np.asarray(r["y_out"]).reshape(NSEQ, S, DM) for r in res.results]
    return np.concatenate(outs, axis=0).astype(np.float32)
```

```python
import contextlib
import numpy as np
import ml_dtypes
import concourse.bass as bass
import concourse.mybir as mybir
from concourse.bass_utils import run_bass_kernel_spmd

F32 = mybir.dt.float32
BF16 = mybir.dt.bfloat16
AF = mybir.ActivationFunctionType
ALU = mybir.AluOpType
AX = mybir.AxisListType

SAME_RAW = True
SEM_ROT = 30000

DM = 1024
MEM = 256
EPS = 1e-6
O_FQ, O_FK, O_FV, O_FF, O_GQ, O_GK, O_GV, O_GG, O_GA, O_END = 0, 512, 1024, 1536, 1544, 1800, 2056, 2568, 3080, 3096
NEXP = 16
DEXP = 512
NEG = -30000.0


class Buf:
    __slots__ = ("name", "w", "r")

    def __init__(self, name=""):
        self.name = name
        self.w = None
        self.r = {}


class Tile:
    def __init__(self, t, name=""):
        self.t = t
        self.b = Buf(name)

    def __getitem__(self, k):
        return self.t[k]


class _Queue:
    def __init__(self, name):
        self.name = name
        self.items = []
        self.sem = None
        self.cnt = 0
        self.semidx = 0
        self.seen = {}
        self.pending = False


class Prog:
    def __init__(self, nc, stack, ndma=8):
        self.nc = nc
        self.stack = stack
        self.q = {n: _Queue(n) for n in ("pe", "act", "dve", "pool", "sp")}
        for q in self.q.values():
            self._newsem(q)
        self.dsem = {}
        for qn in ("sp", "pool", "act"):
            sems = [stack.enter_context(nc.semaphore(f"d_{qn}_{i}")) for i in range(ndma)]
            self.dsem[qn] = {"sems": sems, "cnt": [0] * ndma, "idx": 0}

    def _newsem(self, q):
        q.sem = self.stack.enter_context(self.nc.semaphore(f"s_{q.name}_{q.semidx}"))
        q.semidx += 1
        q.cnt = 0

    def _deps(self, q, qn, reads, writes, is_dma):
        toks = []
        for b in reads:
            if b.w is not None:
                toks.append((b.w, "raw"))
        for b in writes:
            if b.w is not None:
                toks.append((b.w, "waw"))
            for t in b.r.values():
                toks.append((t, "war"))
        waits = []
        for (sem, cnt, src), kind in toks:
            if (not is_dma) and src == qn:
                if qn == "pe":
                    continue
                if not SAME_RAW:
                    continue
            if q.seen.get(sem, 0) >= cnt:
                continue
            q.seen[sem] = cnt
            waits.append((sem, cnt))
        return waits

    def _mark(self, tok, reads, writes):
        for b in writes:
            b.w = tok
            b.r = {}
        for b in reads:
            if b not in writes:
                b.r[tok[0]] = tok

    def op(self, qn, fn, reads=(), writes=(), signal=True):
        q = self.q[qn]
        waits = self._deps(q, qn, reads, writes, False)
        if signal:
            if q.cnt >= SEM_ROT and not q.pending:
                self._newsem(q)
            q.cnt += 1
            tok = (q.sem, q.cnt, qn)
            q.pending = False
            q.items.append((waits, fn, ("inc", q.sem)))
        else:
            tok = (q.sem, q.cnt + 1, qn)
            q.pending = True
            q.items.append((waits, fn, None))
        self._mark(tok, reads, writes)
        return tok

    def dma(self, qn, out, in_, reads=(), writes=(), **kw):
        q = self.q[qn]
        pool = self.dsem[qn]
        i = pool["idx"]
        pool["idx"] = (i + 1) % len(pool["sems"])
        sem = pool["sems"][i]
        prev = pool["cnt"][i]
        waits = self._deps(q, qn, reads, writes, True)
        if prev > 0 and q.seen.get(sem, 0) < prev:
            q.seen[sem] = prev
            waits.append((sem, prev))
        pool["cnt"][i] = prev + 16
        tok = (sem, prev + 16, qn + "_dma")
        q.items.append((waits, (lambda h: h.dma_start(out=out, in_=in_, **kw)), ("dma", sem)))
        self._mark(tok, reads, writes)
        return tok

    def barrier(self):
        toks = []
        for qn, q in self.q.items():
            if q.pending:
                raise RuntimeError("barrier with pending unsignaled op on " + qn)
            if q.cnt > 0:
                toks.append((q.sem, q.cnt, qn))
        for qn, pool in self.dsem.items():
            for s, c in zip(pool["sems"], pool["cnt"]):
                if c > 0:
                    toks.append((s, c, qn + "_dma"))
        for qn, q in self.q.items():
            waits = []
            for sem, cnt, src in toks:
                if src == qn:
                    continue
                if q.seen.get(sem, 0) >= cnt:
                    continue
                q.seen[sem] = cnt
                waits.append((sem, cnt))
            if waits:
                q.items.append((waits, None, None))

    def emit(self):
        self.barrier()
        nc = self.nc

        def runner(q):
            def f(h):
                for waits, fn, sig in q.items:
                    for sem, cnt in waits:
                        h.wait_ge(sem, cnt)
                    if fn is None:
                        continue
                    ins = fn(h)
                    if sig is None:
                        continue
                    if sig[0] == "dma":
                        ins.then_inc(sig[1], 16)
                    else:
                        ins.then_inc(sig[1], 1)
            return f

        with nc.Block() as block:
            block.tensor(runner(self.q["pe"]))
            block.scalar(runner(self.q["act"]))
            block.vector(runner(self.q["dve"]))
            block.gpsimd(runner(self.q["pool"]))
            block.sync(runner(self.q["sp"]))

    def stats(self):
        return {n: len(q.items) for n, q in self.q.items()}


class Ctx:
    pass


def build(cfg):
    S = cfg["S"]
    NSEQ = cfg["NSEQ"]
    L = cfg["L"]
    phases = cfg.get("phases", "ABCDE")
    debug = cfg.get("debug", False)
    T = S * NSEQ
    NT = S // 128
    NG = S // 512
    nc = bass.Bass("TRN2", target_bir_lowering=False)
    C = Ctx()
    C.nc, C.S, C.NSEQ, C.L, C.T, C.NT, C.NG = nc, S, NSEQ, L, T, NT, NG
    C.cfg = cfg

    def din(name, shape, dt=F32):
        return nc.dram_tensor(name, list(shape), dt, kind="ExternalInput").ap()

    def dscr(name, shape, dt):
        return nc.dram_tensor(name, list(shape), dt, kind=("ExternalOutput" if debug else "Internal")).ap()

    I = {}
    I["x"] = din("x", [T, DM])
    I["mem"] = din("mem", [NSEQ * MEM, DM])
    I["mem_norm"] = din("mem_norm", [DM])
    I["mix_norm"] = din("mix_norm", [L, DM])
    I["w_in"] = din("w_in", [L, DM, O_END])
    I["b_forget"] = din("b_forget", [L, 8])
    I["w_alpha_up"] = din("w_alpha_up", [L, 16, 256])
    I["b_alpha"] = din("b_alpha", [L, 256])
    I["fox_out_gain"] = din("fox_out_gain", [L, 512])
    I["gla_out_gain"] = din("gla_out_gain", [L, 512])
    I["w_out"] = din("w_out", [L, DM, DM])
    I["cross_norm"] = din("cross_norm", [L, DM])
    I["w_xq"] = din("w_xq", [L, DM, 512])
    I["w_xk"] = din("w_xk", [L, DM, 512])
    I["w_xv"] = din("w_xv", [L, DM, 512])
    I["w_xo"] = din("w_xo", [L, 512, DM])
    I["moe_norm"] = din("moe_norm", [L, DM])
    I["w_rg"] = din("w_router_group", [L, DM, 4])
    I["b_rg"] = din("b_router_group", [L, 4])
    I["w_re"] = din("w_router_expert", [L, DM, 16])
    I["b_re"] = din("b_router_expert", [L, 16])
    I["w_eg"] = din("w_expert_gate", [L, NEXP, DM, DEXP])
    I["w_eu"] = din("w_expert_up", [L, NEXP, DM, DEXP])
    I["w_ed"] = din("w_expert_down", [L, NEXP, DEXP, DM])
    I["final_norm"] = din("final_norm", [DM])
    I["c_ident"] = din("c_ident", [128, 128])
    I["c_maskadd"] = din("c_maskadd", [128, 128])
    I["c_mask01"] = din("c_mask01", [128, 128])
    I["c_maskq"] = din("c_maskq", [128, 4, 512], BF16)
    C.I = I
    out = nc.dram_tensor("y_out", [T, DM], F32, kind="ExternalOutput").ap()
    C.out = out

    D_ = {}
    D_["h"] = dscr("s_h", [T, DM], F32)
    D_["h2"] = dscr("s_h2", [T, DM], F32)
    C.hcur = I["x"]
    D_["QT"] = dscr("s_QT", [NSEQ, 4, 128, S], BF16)
    D_["KT"] = dscr("s_KT", [NSEQ, 4, 128, S], BF16)
    D_["vaug"] = dscr("s_vaug", [T, 520], BF16)
    D_["AB"] = dscr("s_AB", [NSEQ, 8, 6, S], BF16)
    D_["qgT"] = dscr("s_qgT", [NSEQ, 64, 4, S], BF16)
    D_["kgT"] = dscr("s_kgT", [NSEQ, 64, 4, S], BF16)
    D_["ktok"] = dscr("s_ktok", [T, 256], BF16)
    D_["gv"] = dscr("s_gv", [T, 512], BF16)
    D_["gg"] = dscr("s_gg", [T, 512], BF16)
    D_["ebl"] = dscr("s_ebl", [NSEQ, 64, 4, NT], F32)
    D_["ymix"] = dscr("s_ymix", [T, DM], BF16)
    C.D = D_

    with contextlib.ExitStack() as st:
        P = Prog(nc, st)
        C.P = P

        def gsb(name, shape, dt):
            return Tile(st.enter_context(nc.sbuf_tensor("g_" + name, shape, dt)), name)

        C.banks = [Tile(st.enter_context(nc.psum_tensor(f"bank{i}", [128, 512], F32)), f"bank{i}") for i in range(8)]
        C.bank_i = 0
        C.idf = gsb("idf", [128, 128], F32)
        C.idb = gsb("idb", [128, 128], BF16)
        C.maskadd = gsb("maskadd", [128, 128], BF16)
        C.mask01 = gsb("mask01", [128, 128], F32)
        C.ones = gsb("ones", [128, 512], F32)
        C.memnT = gsb("memnT", [128, 8, NSEQ * MEM], BF16)
        tmpc = gsb("tmpc", [128, 128], F32)
        P.dma("sp", C.idf[:], I["c_ident"], writes=[C.idf.b])
        P.op("act", lambda h: h.copy(out=C.idb[:], in_=C.idf[:]), reads=[C.idf.b], writes=[C.idb.b])
        P.dma("sp", tmpc[:], I["c_maskadd"], writes=[tmpc.b])
        P.op("act", lambda h: h.copy(out=C.maskadd[:], in_=tmpc[:]), reads=[tmpc.b], writes=[C.maskadd.b])
        P.dma("sp", C.mask01[:], I["c_mask01"], writes=[C.mask01.b])
        P.op("dve", lambda h: h.memset(C.ones[:], 1.0), writes=[C.ones.b])
        C.maskq = gsb("maskq", [128, 4, 512], BF16)
        P.dma("sp", C.maskq[:], I["c_maskq"], writes=[C.maskq.b])

        if "0" in phases:
            phase_mem(C)
        for l in range(L):
            if "A" in phases:
                phase_A(C, l)
            if "B" in phases or "1" in phases:
                phase_B1(C, l)
            if "B" in phases or "2" in phases:
                phase_B2(C, l)
            if "B" in phases or "3" in phases:
                phase_B3(C, l)
            if "C" in phases:
                phase_C(C, l)
            if "D" in phases:
                phase_D(C, l, last=(l == L - 1))
        P.emit()
        C.stats = P.stats()
    return nc, C


def next_bank(C):
    b = C.banks[C.bank_i]
    C.bank_i = (C.bank_i + 1) % 8
    return b


def bf16view(bank):
    return bank.t[:].bitcast(BF16)


def MM(C, out, lhsT, rhs, start, stop, reads, writes, signal=True):
    C.P.op("pe", lambda h: h.matmul(out=out, lhsT=lhsT, rhs=rhs, start=start, stop=stop), reads, writes, signal)


def TR(C, out, in_, ident, reads, writes, signal=True):
    C.P.op("pe", lambda h: h.transpose(out=out, in_=in_, identity=ident), reads, writes, signal)


def ACT(C, out, in_, func, reads, writes, **kw):
    C.P.op("act", lambda h: h.activation(out=out, in_=in_, func=func, **kw), reads, writes)


def TT(C, out, in0, in1, op, reads, writes):
    C.P.op("dve", lambda h: h.tensor_tensor(out=out, in0=in0, in1=in1, op=op), reads, writes)


def TS(C, out, in0, s1, s2, op0, op1, reads, writes):
    if s2 is None:
        C.P.op("dve", lambda h: h.tensor_scalar(out=out, in0=in0, scalar1=s1, scalar2=None, op0=op0), reads, writes)
    else:
        C.P.op("dve", lambda h: h.tensor_scalar(out=out, in0=in0, scalar1=s1, scalar2=s2, op0=op0, op1=op1), reads, writes)


def STT(C, out, in0, scalar, in1, op0, op1, reads, writes):
    C.P.op("dve", lambda h: h.scalar_tensor_tensor(out=out, in0=in0, scalar=scalar, in1=in1, op0=op0, op1=op1), reads, writes)


def CP(C, eng, out, in_, reads, writes, scale=None):
    if eng == "act":
        if scale is None:
            C.P.op("act", lambda h: h.copy(out=out, in_=in_), reads, writes)
        else:
            C.P.op("act", lambda h: h.mul(out=out, in_=in_, mul=scale), reads, writes)
    else:
        if scale is None:
            C.P.op("dve", lambda h: h.tensor_copy(out=out, in_=in_), reads, writes)
        else:
            TS(C, out, in_, scale, None, ALU.mult, None, reads, writes)


def MS(C, ap, val, writes):
    C.P.op("dve", lambda h: h.memset(ap, val), (), writes)


def RED(C, out, in_, op, reads, writes):
    C.P.op("dve", lambda h: h.tensor_reduce(out=out, in_=in_, axis=AX.X, op=op), reads, writes)


def RCP(C, out, in_, reads, writes):
    C.P.op("dve", lambda h: h.reciprocal(out=out, in_=in_), reads, writes)


def SCAN(C, out, d0, d1, init, reads, writes):
    C.P.op("dve", lambda h: h.tensor_tensor_scan(out=out, data0=d0, data1=d1, initial=init, op0=ALU.mult, op1=ALU.add), reads, writes)


def DMA(C, out, in_, reads=(), writes=(), **kw):
    C.P.dma("sp", out, in_, reads, writes, **kw)


def rstd_ops(C, ss, ln, rstd, inv_n):
    ACT(C, ln[0], ss[0], AF.Ln, [ss[1]], [ln[1]], scale=inv_n, bias=EPS)
    ACT(C, rstd[0], ln[0], AF.Exp, [ln[1]], [rstd[1]], scale=-0.5)


def mm_group(C, out_ap, bank, pairs, reads):
    n = len(pairs)
    for k, (a, b) in enumerate(pairs):
        MM(C, out_ap, a, b, k == 0, k == n - 1, reads, [bank.b], signal=(k == n - 1))


def load_weight_bf16(C, dst_tile, dst_aps, src_aps, stage_tiles):
    for k, (dap, src) in enumerate(zip(dst_aps, src_aps)):
        stg = stage_tiles[k % len(stage_tiles)]
        shp = list(src.shape)
        sap = stg.t[:]
        if list(sap.shape) != shp:
            if len(shp) == 2:
                sap = stg.t[0:shp[0], 0:shp[1]]
            else:
                sap = stg.t[0:shp[0], 0:shp[1] * shp[2]].rearrange("p (a b) -> p a b", a=shp[1])
        DMA(C, sap, src, writes=[stg.b])
        CP(C, "act", dap, sap, [stg.b], [dst_tile.b])


def norm_tile(C, src_dram_ap, xt, junk, small, gain_bc, out_ap, out_buf):
    ss, ln, rstd = small
    DMA(C, xt[:], src_dram_ap, writes=[xt.b])
    ACT(C, junk[:], xt[:], AF.Square, [xt.b], [junk.b, ss.b], accum_out=ss[:])
    rstd_ops(C, (ss[:], ss.b), (ln[:], ln.b), (rstd[:], rstd.b), 1.0 / DM)
    STT(C, out_ap, xt[:], rstd[:], gain_bc[:], ALU.mult, ALU.mult, [xt.b, rstd.b, gain_bc.b], [out_buf])


def norm_transpose_tile(C, src_dram_ap, xt, junk, small, hn, gain_bc, hnT, col0):
    norm_tile(C, src_dram_ap, xt, junk, small, gain_bc, hn[:], hn.b)
    bank = next_bank(C)
    bv = bf16view(bank)
    for k in range(8):
        TR(C, bv[:, k * 128:(k + 1) * 128], hn[:, k * 128:(k + 1) * 128], C.idb[:], [hn.b, C.idb.b], [bank.b], signal=(k == 7))
    CP(C, "dve", hnT[:, :, col0:col0 + 128], bv.rearrange("p (k t) -> p k t", k=8), [bank.b], [hnT.b])


def norm_scratch(sb):
    xts = [sb(f"xt{i}", [128, DM], F32) for i in range(2)]
    junk = sb("junk", [128, DM], BF16)
    hns = [sb(f"hn{i}", [128, DM], BF16) for i in range(2)]
    smalls = [[sb(f"{n}{i}", [128, 1], F32) for n in ("ss", "ln", "rstd")] for i in range(2)]
    return xts, junk, hns, smalls


def phase_mem(C):
    nc, P, I = C.nc, C.P, C.I
    with contextlib.ExitStack() as ph:
        def sb(name, shape, dt):
            return Tile(ph.enter_context(nc.sbuf_tensor("M_" + name, shape, dt)), name)
        gain = sb("gain", [128, DM], F32)
        DMA(C, gain[:], I["mem_norm"].partition_broadcast(128), writes=[gain.b])
        xts, junk, hns, smalls = norm_scratch(sb)
        for i in range(C.NSEQ * MEM // 128):
            norm_transpose_tile(C, I["mem"][i * 128:(i + 1) * 128, :], xts[i % 2], junk, smalls[i % 2], hns[i % 2],
                                gain, C.memnT, i * 128)
        P.barrier()


def phase_A(C, l):
    nc, P, I, D_ = C.nc, C.P, C.I, C.D
    S, NSEQ, NG, NT = C.S, C.NSEQ, C.NG, C.NT
    hsrc = C.hcur
    with contextlib.ExitStack() as ph:
        def sb(name, shape, dt):
            return Tile(ph.enter_context(nc.sbuf_tensor(f"A{l}_" + name, shape, dt)), name)
        win = sb("win", [128, 8, O_END], BF16)
        HW_ = O_END // 2
        wst = [sb(f"wst{i}", [128, HW_], F32) for i in range(2)]
        load_weight_bf16(C, win, [win[:, k // 2, (k % 2) * HW_:(k % 2 + 1) * HW_] for k in range(16)],
                         [I["w_in"][l, (k // 2) * 128:(k // 2 + 1) * 128, (k % 2) * HW_:(k % 2 + 1) * HW_] for k in range(16)], wst)
        wup = sb("wup", [16, 256], BF16)
        load_weight_bf16(C, wup, [wup[:]], [I["w_alpha_up"][l]], wst)
        gain = sb("gain", [128, DM], F32)
        DMA(C, gain[:], I["mix_norm"][l].partition_broadcast(128), writes=[gain.b])
        nba = sb("nba", [64, 4], F32)
        nbf = sb("nbf", [8, 1], F32)
        DMA(C, nba[:], I["b_alpha"][l].rearrange("(h d) -> d h", d=64), writes=[nba.b], allow_slow_non_contiguous=True)
        DMA(C, nbf[:], I["b_forget"][l].rearrange("(h o) -> h o", o=1), writes=[nbf.b])
        CP(C, "act", nba[:], nba[:], [nba.b], [nba.b], scale=-1.0)
        CP(C, "act", nbf[:], nbf[:], [nbf.b], [nbf.b], scale=-1.0)
        ones8 = sb("ones8", [8, 512], F32)
        MS(C, ones8[:], 1.0, [ones8.b])
        xts, junk, hns, smalls = norm_scratch(sb)
        hnTs = [sb(f"hnT{i}", [128, 8, 512], BF16) for i in range(2)]
        stqs = [sb(f"stq{i}", [128, 8, 512], BF16) for i in range(1)] * 2
        stvs = [sb(f"stv{i}", [128, 4, 8, 65], BF16) for i in range(2)]
        for t_ in stvs:
            MS(C, t_[:], 1.0, [t_.b])
        stgv = [sb(f"stgv{i}", [128, 4, 512], BF16) for i in range(1)] * 2
        stgg = [sb(f"stgg{i}", [128, 4, 512], BF16) for i in range(1)] * 2
        gaT = sb("gaT", [16, 512], BF16)
        e1 = sb("e1", [64, 4, 512], F32)
        LgG = sb("LgG", [64, 4, 513], F32)
        Dg = sb("Dg", [64, 4, 512], F32)
        expb = sb("expb", [64, 4, 512], F32)
        expnb = e1
        qts = [sb(f"qgt{i}", [64, 4, 512], BF16) for i in range(2)]
        kts = [sb(f"kgt{i}", [64, 4, 512], BF16) for i in range(2)]
        ktoks = [sb(f"ktok{i}", [128, 4, 256], BF16) for i in range(2)]
        ebl = sb("ebl", [64, 4, NT], F32)
        ef = sb("ef", [8, 512], F32)
        lf = ef
        Lf = sb("Lf", [8, 513], F32)
        r1 = sb("r1", [8, 512], F32)
        ABst = sb("ABst", [8, 6, 512], BF16)
        P.barrier()

        gi = 0
        for b in range(NSEQ):
            MS(C, LgG[:, :, 0:1], 0.0, [LgG.b])
            MS(C, Lf[:, 0:1], 0.0, [Lf.b])
            for g in range(NG):
                hnT, stq, stv, sgv, sgg = hnTs[gi % 2], stqs[gi % 2], stvs[gi % 2], stgv[gi % 2], stgg[gi % 2]
                qt, kt, ktok = qts[gi % 2], kts[gi % 2], ktoks[gi % 2]
                gi += 1
                tg = b * S + g * 512
                gsl = slice(g * 512, (g + 1) * 512)
                for i in range(4):
                    t0 = tg + i * 128
                    norm_transpose_tile(C, hsrc[t0:t0 + 128, :], xts[i % 2], junk, smalls[i % 2], hns[i % 2], gain, hnT, i * 128)
                for c in range(8):
                    col = (O_FQ if c < 4 else O_FK) + (c % 4) * 128
                    bank = next_bank(C)
                    mm_group(C, bank[:], bank, [(win[:, k, col:col + 128], hnT[:, k, :]) for k in range(8)], [win.b, hnT.b])
                    CP(C, "act" if c % 2 == 0 else "dve", stq[:, c, :], bank[:], [bank.b], [stq.b], scale=(0.125 if c < 4 else None))
                DMA(C, D_["QT"][b, :, :, gsl].rearrange("c p t -> p c t"), stq[:, 0:4, :], reads=[stq.b])
                DMA(C, D_["KT"][b, :, :, gsl].rearrange("c p t -> p c t"), stq[:, 4:8, :], reads=[stq.b])
                bank = next_bank(C)
                mm_group(C, bank[0:8, :], bank, [(win[:, k, O_FF:O_FF + 8], hnT[:, k, :]) for k in range(8)], [win.b, hnT.b])
                ACT(C, ef[:], bank[0:8, :], AF.Exp, [bank.b, nbf.b], [ef.b], scale=-1.0, bias=nbf[:])
                ACT(C, lf[:], ef[:], AF.Ln, [ef.b], [lf.b], bias=1.0)
                SCAN(C, Lf[:, 1:513], ones8[:], lf[:], Lf[:, 0:1], [ones8.b, lf.b, Lf.b], [Lf.b])
                CP(C, "dve", ABst[:, 3, :], Lf[:, 1:513], [Lf.b], [ABst.b])
                TT(C, r1[:], Lf[:, 1:513], ABst[:, 3, :], ALU.subtract, [Lf.b, ABst.b], [r1.b])
                CP(C, "dve", ABst[:, 4, :], r1[:], [r1.b], [ABst.b])
                TT(C, r1[:], r1[:], ABst[:, 4, :], ALU.subtract, [r1.b, ABst.b], [r1.b])
                CP(C, "dve", ABst[:, 5, :], r1[:], [r1.b], [ABst.b])
                CP(C, "act", ABst[:, 0:3, :], ABst[:, 3:6, :], [ABst.b], [ABst.b], scale=-1.0)
                DMA(C, D_["AB"][b, :, :, gsl], ABst[:], reads=[ABst.b])
                CP(C, "dve", Lf[:, 0:1], Lf[:, 512:513], [Lf.b], [Lf.b])
                bank = next_bank(C)
                mm_group(C, bank[0:16, :], bank, [(win[:, k, O_GA:O_GA + 16], hnT[:, k, :]) for k in range(8)], [win.b, hnT.b])
                CP(C, "act", gaT[:], bank[0:16, :], [bank.b], [gaT.b])
                for hh in range(4):
                    bank = next_bank(C)
                    mm_group(C, bank[0:64, :], bank, [(wup[:, hh * 64:(hh + 1) * 64], gaT[:])], [wup.b, gaT.b])
                    ACT(C, e1[:, hh, :], bank[0:64, :], AF.Exp, [bank.b, nba.b], [e1.b], scale=-1.0, bias=nba[:, hh:hh + 1])
                ACT(C, e1[:], e1[:], AF.Ln, [e1.b], [e1.b], bias=1.0)
                for hh in range(4):
                    SCAN(C, LgG[:, hh, 1:513], C.ones[0:64, :], e1[:, hh, :], LgG[:, hh, 0:1], [C.ones.b, e1.b, LgG.b], [LgG.b])
                TT(C, Dg[:].rearrange("p h (c t) -> p h c t", t=128),
                   LgG[:, :, 1:513].rearrange("p h (c t) -> p h c t", t=128),
                   LgG[:, :, 0:512].rearrange("p h (c t) -> p h c t", t=128)[:, :, :, 0:1].to_broadcast([64, 4, 4, 128]),
                   ALU.subtract, [LgG.b], [Dg.b])
                ACT(C, expb[:], Dg[:], AF.Exp, [Dg.b], [expb.b], scale=-1.0 / 16)
                ACT(C, expnb[:], Dg[:], AF.Exp, [Dg.b], [expnb.b], scale=1.0 / 16)
                CP(C, "dve", ebl[:, :, g * 4:(g + 1) * 4], expb[:].rearrange("p h (c t) -> p h c t", t=128)[:, :, :, 127], [expb.b], [ebl.b])
                CP(C, "dve", LgG[:, :, 0:1], LgG[:, :, 512:513], [LgG.b], [LgG.b])
                for hh in range(4):
                    bank = next_bank(C)
                    col = O_GQ + hh * 64
                    mm_group(C, bank[0:64, :], bank, [(win[:, k, col:col + 64], hnT[:, k, :]) for k in range(8)], [win.b, hnT.b])
                    STT(C, qt[:, hh, :], bank[0:64, :], 0.125, expb[:, hh, :], ALU.mult, ALU.mult, [bank.b, expb.b], [qt.b])
                    bank = next_bank(C)
                    col = O_GK + hh * 64
                    mm_group(C, bank[0:64, :], bank, [(win[:, k, col:col + 64], hnT[:, k, :]) for k in range(8)], [win.b, hnT.b])
                    TT(C, kt[:, hh, :], bank[0:64, :], expnb[:, hh, :], ALU.mult, [bank.b, expnb.b], [kt.b])
                DMA(C, D_["qgT"][b, :, :, gsl], qt[:], reads=[qt.b])
                DMA(C, D_["kgT"][b, :, :, gsl], kt[:], reads=[kt.b])
                bank = next_bank(C)
                bv = bf16view(bank)
                for c in range(4):
                    for hh in range(4):
                        o0 = c * 256 + hh * 64
                        TR(C, bv[:, o0:o0 + 64], kt[:, hh, c * 128:(c + 1) * 128], C.idb[0:64, 0:64], [kt.b, C.idb.b], [bank.b],
                           signal=(c == 3 and hh == 3))
                CP(C, "act", ktok[:], bv.rearrange("p (c f) -> p c f", c=4), [bank.b], [ktok.b])
                DMA(C, D_["ktok"][tg:tg + 512, :].rearrange("(c p) f -> p c f", p=128), ktok[:], reads=[ktok.b])
                for i in range(4):
                    lhs = [hnT[:, k, i * 128:(i + 1) * 128] for k in range(8)]
                    bank = next_bank(C)
                    mm_group(C, bank[:], bank, [(lhs[k], win[:, k, O_FV:O_FV + 512]) for k in range(8)], [win.b, hnT.b])
                    CP(C, "dve", stv[:, i, :, 0:64], bank[:].rearrange("p (h d) -> p h d", d=64), [bank.b], [stv.b])
                    bank = next_bank(C)
                    mm_group(C, bank[:], bank, [(lhs[k], win[:, k, O_GV:O_GV + 512]) for k in range(8)], [win.b, hnT.b])
                    CP(C, "act", sgv[:, i, :], bank[:], [bank.b], [sgv.b])
                    bank = next_bank(C)
                    mm_group(C, bank[:], bank, [(lhs[k], win[:, k, O_GG:O_GG + 512]) for k in range(8)], [win.b, hnT.b])
                    ACT(C, sgg[:, i, :], bank[:], AF.Silu, [bank.b], [sgg.b])
                DMA(C, D_["vaug"][tg:tg + 512, :].rearrange("(i p) f -> p i f", p=128), stv[:].rearrange("p i h d -> p i (h d)"), reads=[stv.b])
                DMA(C, D_["gv"][tg:tg + 512, :].rearrange("(i p) f -> p i f", p=128), sgv[:], reads=[sgv.b])
                DMA(C, D_["gg"][tg:tg + 512, :].rearrange("(i p) f -> p i f", p=128), sgg[:], reads=[sgg.b])
            DMA(C, D_["ebl"][b], ebl[:], reads=[ebl.b])
        P.barrier()


def phase_B1(C, l):
    nc, P, I, D_ = C.nc, C.P, C.I, C.D
    S, NSEQ, NT = C.S, C.NSEQ, C.NT
    NJ = NT // 4
    with contextlib.ExitStack() as ph:
        def sb(name, shape, dt):
            return Tile(ph.enter_context(nc.sbuf_tensor(f"B1{l}_" + name, shape, dt)), name)
        QT = sb("QT", [128, 4, S], BF16)
        KT = sb("KT", [128, 4, S], BF16)
        Va = sb("Va", [128, NT, 520], BF16)
        Ars = [sb(f"Ar{i}", [128, S], BF16) for i in range(2)]
        Brs = [sb(f"Br{i}", [128, S], BF16) for i in range(2)]
        for t_ in Ars + Brs:
            MS(C, t_[:], 0.0, [t_.b])
        for k_ in range(2):
            MS(C, Ars[k_][k_ * 64:k_ * 64 + 6, :], 1.0, [Ars[k_].b])
            MS(C, Brs[k_][k_ * 64:k_ * 64 + 6, :], 1.0, [Brs[k_].b])
        PTs = [sb(f"PT{i}", [128, 512], BF16) for i in range(4)]
        oTs = [sb(f"oT{i}", [65, 512], BF16) for i in range(2)]
        rec4s = [sb(f"rec4{i}", [128, 4], F32) for i in range(2)]
        fo = sb("fo", [128, NT, 512], F32)
        gain = sb("gain", [128, 512], F32)
        DMA(C, gain[:], I["fox_out_gain"][l].partition_broadcast(128), writes=[gain.b])
        tmp = sb("tmp", [128, 512], F32)
        ssq = sb("ssq", [128, 8], F32)
        lnq = sb("lnq", [128, 8], F32)
        rsq = sb("rsq", [128, 8], F32)
        ybs = [sb(f"yb{i}", [128, 512], BF16) for i in range(2)]
        P.barrier()
        sbanks = C.banks[0:4]
        abanks = C.banks[4:6]
        tbanks = C.banks[6:8]
        si = ai = hi = pi = 0
        for b in range(NSEQ):
            DMA(C, QT[:], D_["QT"][b].rearrange("c p t -> p c t"), writes=[QT.b])
            DMA(C, KT[:], D_["KT"][b].rearrange("c p t -> p c t"), writes=[KT.b])
            DMA(C, Va[:], D_["vaug"][b * S:(b + 1) * S, :].rearrange("(i p) f -> p i f", p=128), writes=[Va.b])
            for hh in range(8):
                c = hh // 2
                pr = (hh % 2) * 64
                Ar, Br = Ars[hh % 2], Brs[hh % 2]
                DMA(C, Ar[pr + 3:pr + 6, :], D_["AB"][b, hh, 3:6, :], writes=[Ar.b])
                DMA(C, Br[pr:pr + 3, :], D_["AB"][b, hh, 0:3, :], writes=[Br.b])
                for J in range(NJ):
                    tq = slice(J * 512, (J + 1) * 512)
                    nI = 4 * J + 4
                    ab = abanks[ai % 2]
                    oT = oTs[ai % 2]
                    rec4 = rec4s[ai % 2]
                    tb = tbanks[ai % 2]
                    ai += 1
                    pend = None
                    for i in range(nI + 1):
                        if i < nI:
                            ks = slice(i * 128, (i + 1) * 128)
                            bank = sbanks[si % 4]
                            si += 1
                            PT = PTs[pi % 4]
                            pi += 1
                            diag = (i >= 4 * J)
                            MM(C, bank[:], KT[pr:pr + 64, c, ks], QT[pr:pr + 64, c, tq], True, False, [KT.b, QT.b], [bank.b], signal=False)
                            MM(C, bank[:], Ar[pr:pr + 64, ks], Br[pr:pr + 64, tq], False, not diag, [Ar.b, Br.b], [bank.b], signal=(not diag))
                            if diag:
                                MM(C, bank[:], C.idb[:], C.maskq[:, i - 4 * J, :], False, True, [C.idb.b, C.maskq.b], [bank.b], signal=True)
                            ACT(C, PT[:], bank[:], AF.Exp, [bank.b], [PT.b])
                        if pend is not None:
                            ip, PTp = pend
                            MM(C, ab[0:65, :], Va[:, ip, hh * 65:(hh + 1) * 65], PTp[:], ip == 0, ip == nI - 1, [Va.b, PTp.b], [ab.b],
                               signal=(ip == nI - 1))
                        pend = (i, PT) if i < nI else None
                    CP(C, "act", oT[:], ab[0:65, :], [ab.b], [oT.b])
                    tbv = bf16view(tb)
                    for jj in range(4):
                        TR(C, tbv[:, jj * 128:jj * 128 + 65], oT[:, jj * 128:(jj + 1) * 128], C.idb[0:65, 0:65], [oT.b, C.idb.b], [tb.b],
                           signal=(jj == 3))
                    tb3 = tbv[:, 0:512].rearrange("p (j f) -> p j f", f=128)
                    RCP(C, rec4[:], tb3[:, :, 64], [tb.b], [rec4.b])
                    TT(C, fo[:, 4 * J:4 * J + 4, hh * 64:(hh + 1) * 64], tb3[:, :, 0:64], rec4[:].unsqueeze(2).to_broadcast([128, 4, 64]), ALU.mult,
                       [tb.b, rec4.b], [fo.b])
            for j in range(NT):
                yb = ybs[j % 2]
                TT(C, tmp[:], fo[:, j, :], fo[:, j, :], ALU.mult, [fo.b], [tmp.b])
                RED(C, ssq[:], tmp[:].rearrange("p (h d) -> p h d", d=64), ALU.add, [tmp.b], [ssq.b])
                rstd_ops(C, (ssq[:], ssq.b), (lnq[:], lnq.b), (rsq[:], rsq.b), 1.0 / 64)
                TT(C, tmp[:].rearrange("p (h d) -> p h d", d=64), fo[:, j, :].rearrange("p (h d) -> p h d", d=64),
                   rsq[:].unsqueeze(2).to_broadcast([128, 8, 64]), ALU.mult, [fo.b, rsq.b], [tmp.b])
                TT(C, yb[:], tmp[:], gain[:], ALU.mult, [tmp.b, gain.b], [yb.b])
                r0 = b * S + j * 128
                DMA(C, D_["ymix"][r0:r0 + 128, 0:512], yb[:], reads=[yb.b])
        P.barrier()


def phase_B2(C, l):
    nc, P, I, D_ = C.nc, C.P, C.I, C.D
    S, NSEQ, NT = C.S, C.NSEQ, C.NT
    with contextlib.ExitStack() as ph:
        def sb(name, shape, dt):
            return Tile(ph.enter_context(nc.sbuf_tensor(f"B2{l}_" + name, shape, dt)), name)
        qg = sb("qg", [64, 4, S], BF16)
        kg = sb("kg", [64, 4, S], BF16)
        ktok = sb("ktok", [128, NT, 256], BF16)
        gv = sb("gv", [128, NT, 512], BF16)
        gg = sb("gg", [128, NT, 512], BF16)
        ebl = sb("ebl", [64, 4, NT], F32)
        Sf = sb("Sf", [64, 4, 128], F32)
        Sb_ = sb("Sb", [64, 4, 128], BF16)
        tS = sb("tS", [64, 4, 128], F32)
        atts = [sb(f"att{i}", [128, 4, 128], BF16) for i in range(2)]
        gain = sb("gain", [128, 512], F32)
        DMA(C, gain[:], I["gla_out_gain"][l].partition_broadcast(128), writes=[gain.b])
        sq = sb("sq", [128, 512], F32)
        ssq = sb("ssq", [128, 4], F32)
        lnq = sb("lnq", [128, 4], F32)
        rsq = sb("rsq", [128, 4], F32)
        t1 = sb("t1", [128, 512], F32)
        ybs = [sb(f"yb{i}", [128, 512], BF16) for i in range(2)]
        for b in range(NSEQ):
            DMA(C, qg[:], D_["qgT"][b], writes=[qg.b])
            DMA(C, kg[:], D_["kgT"][b], writes=[kg.b])
            DMA(C, ktok[:], D_["ktok"][b * S:(b + 1) * S, :].rearrange("(i p) f -> p i f", p=128), writes=[ktok.b])
            DMA(C, gv[:], D_["gv"][b * S:(b + 1) * S, :].rearrange("(i p) f -> p i f", p=128), writes=[gv.b])
            DMA(C, gg[:], D_["gg"][b * S:(b + 1) * S, :].rearrange("(i p) f -> p i f", p=128), writes=[gg.b])
            DMA(C, ebl[:], D_["ebl"][b], writes=[ebl.b])
            MS(C, Sf[:], 0.0, [Sf.b])
            MS(C, Sb_[:], 0.0, [Sb_.b])
            for c in range(NT):
                att = atts[c % 2]
                yb = ybs[c % 2]
                cs = slice(c * 128, (c + 1) * 128)
                bk_a = next_bank(C)
                for hh in range(4):
                    MM(C, bk_a[:, hh * 128:(hh + 1) * 128], kg[:, hh, cs], qg[:, hh, cs], True, True, [kg.b, qg.b], [bk_a.b], signal=(hh == 3))
                TT(C, att[:], bk_a[:].rearrange("p (h t) -> p h t", t=128), C.mask01[:].unsqueeze(1).to_broadcast([128, 4, 128]), ALU.mult,
                   [bk_a.b, C.mask01.b], [att.b])
                bk_o = next_bank(C)
                for hh in range(4):
                    o = bk_o[:, hh * 128:(hh + 1) * 128]
                    MM(C, o, qg[:, hh, cs], Sb_[:, hh, :], True, False, [qg.b, Sb_.b], [bk_o.b], signal=False)
                    MM(C, o, att[:, hh, :], gv[:, c, hh * 128:(hh + 1) * 128], False, True, [att.b, gv.b], [bk_o.b], signal=(hh == 3))
                bk_s = next_bank(C)
                for hh in range(4):
                    MM(C, bk_s[0:64, hh * 128:(hh + 1) * 128], ktok[:, c, hh * 64:(hh + 1) * 64], gv[:, c, hh * 128:(hh + 1) * 128], True, True,
                       [ktok.b, gv.b], [bk_s.b], signal=(hh == 3))
                TT(C, tS[:].rearrange("p h v -> p (h v)"), bk_s[0:64, :], Sf[:].rearrange("p h v -> p (h v)"), ALU.add, [bk_s.b, Sf.b], [tS.b])
                TT(C, Sf[:], tS[:], ebl[:, :, c:c + 1].to_broadcast([64, 4, 128]), ALU.mult, [tS.b, ebl.b], [Sf.b])
                CP(C, "act", Sb_[:], Sf[:], [Sf.b], [Sb_.b])
                ACT(C, sq[:], bk_o[:], AF.Square, [bk_o.b], [sq.b])
                RED(C, ssq[:], sq[:].rearrange("p (h d) -> p h d", d=128), ALU.add, [sq.b], [ssq.b])
                rstd_ops(C, (ssq[:], ssq.b), (lnq[:], lnq.b), (rsq[:], rsq.b), 1.0 / 128)
                TT(C, t1[:].rearrange("p (h d) -> p h d", d=128), bk_o[:].rearrange("p (h d) -> p h d", d=128),
                   rsq[:].unsqueeze(2).to_broadcast([128, 4, 128]), ALU.mult, [bk_o.b, rsq.b], [t1.b])
                TT(C, t1[:], t1[:], gain[:], ALU.mult, [t1.b, gain.b], [t1.b])
                TT(C, yb[:], t1[:], gg[:, c, :], ALU.mult, [t1.b, gg.b], [yb.b])
                r0 = b * S + c * 128
                DMA(C, D_["ymix"][r0:r0 + 128, 512:1024], yb[:], reads=[yb.b])
        P.barrier()


def proj_residual(C, l, tagname, ysrc_fn, kchunks, w_dram, hsrc, hdst, ntiles, final_gain=None):
    nc, P = C.nc, C.P
    with contextlib.ExitStack() as ph:
        def sb(name, shape, dt):
            return Tile(ph.enter_context(nc.sbuf_tensor(f"{tagname}{l}_" + name, shape, dt)), name)
        wo = sb("wo", [128, kchunks, DM], BF16)
        wst = [sb(f"wst{i}", [128, DM], F32) for i in range(2)]
        load_weight_bf16(C, wo, [wo[:, k, :] for k in range(kchunks)], [w_dram[k * 128:(k + 1) * 128, :] for k in range(kchunks)], wst)
        yts = [sb(f"yt{i}", [128, kchunks * 128], BF16) for i in range(2)]
        yTs = [sb(f"yT{i}", [128, kchunks, 128], BF16) for i in range(2)]
        hts = [sb(f"ht{i}", [128, DM], F32) for i in range(2)]
        for i in range(ntiles):
            yt, yT, ht = yts[i % 2], yTs[i % 2], hts[i % 2]
            DMA(C, yt[:], ysrc_fn(i), writes=[yt.b])
            DMA(C, ht[:], hsrc[i * 128:(i + 1) * 128, :], writes=[ht.b])
            bank = next_bank(C)
            bv = bf16view(bank)
            for k in range(kchunks):
                TR(C, bv[:, k * 128:(k + 1) * 128], yt[:, k * 128:(k + 1) * 128], C.idb[:], [yt.b, C.idb.b], [bank.b], signal=(k == kchunks - 1))
            CP(C, "act", yT[:], bv[:, 0:kchunks * 128].rearrange("p (k t) -> p k t", k=kchunks), [bank.b], [yT.b])
            for n in range(2):
                bank = next_bank(C)
                mm_group(C, bank[:], bank, [(yT[:, k, :], wo[:, k, n * 512:(n + 1) * 512]) for k in range(kchunks)], [yT.b, wo.b])
                TT(C, ht[:, n * 512:(n + 1) * 512], bank[:], ht[:, n * 512:(n + 1) * 512], ALU.add, [bank.b, ht.b], [ht.b])
            DMA(C, hdst[i * 128:(i + 1) * 128, :], ht[:], reads=[ht.b])
        P.barrier()


def hnext(C):
    return C.D["h2"] if C.hcur is C.D["h"] else C.D["h"]


def phase_B3(C, l):
    dst = hnext(C)
    proj_residual(C, l, "B3", lambda i: C.D["ymix"][i * 128:(i + 1) * 128, :], 8, C.I["w_out"][l], C.hcur, dst, C.T // 128)
    C.hcur = dst


def phase_C(C, l):
    nc, P, I, D_ = C.nc, C.P, C.I, C.D
    S, NSEQ, NG = C.S, C.NSEQ, C.NG
    XS = 128 ** -0.5
    with contextlib.ExitStack() as ph:
        def sb(name, shape, dt):
            return Tile(ph.enter_context(nc.sbuf_tensor(f"C{l}_" + name, shape, dt)), name)
        wst = [sb(f"wst{i}", [128, 4, 512], F32) for i in range(2)]
        wq = sb("wq", [128, 8, 512], BF16)
        wk = sb("wk", [128, 8, 512], BF16)
        wv = sb("wv", [128, 8, 512], BF16)
        for wt, name in ((wk, "w_xk"), (wv, "w_xv"), (wq, "w_xq")):
            src = I[name][l].rearrange("(k p) f -> p k f", p=128)
            load_weight_bf16(C, wt, [wt[:, 0:4, :], wt[:, 4:8, :]], [src[:, 0:4, :], src[:, 4:8, :]], wst)
        gain = sb("gain", [128, DM], F32)
        DMA(C, gain[:], I["cross_norm"][l].partition_broadcast(128), writes=[gain.b])
        KmT = sb("KmT", [128, NSEQ, 4, MEM], BF16)
        Vm = sb("Vm", [128, NSEQ, 2, 4, 129], BF16)
        MS(C, Vm[:], 1.0, [Vm.b])
        for b in range(NSEQ):
            for hh in range(4):
                bank = next_bank(C)
                mm_group(C, bank[:, 0:MEM], bank, [(wk[:, k, hh * 128:(hh + 1) * 128], C.memnT[:, k, b * MEM:(b + 1) * MEM]) for k in range(8)],
                         [wk.b, C.memnT.b])
                CP(C, "act", KmT[:, b, hh, :], bank[:, 0:MEM], [bank.b], [KmT.b], scale=XS)
            for mb in range(2):
                bank = next_bank(C)
                mm_group(C, bank[:], bank, [(C.memnT[:, k, b * MEM + mb * 128:b * MEM + (mb + 1) * 128], wv[:, k, :]) for k in range(8)],
                         [wv.b, C.memnT.b])
                CP(C, "dve", Vm[:, b, mb, :, 0:128], bank[:].rearrange("p (h d) -> p h d", d=128), [bank.b], [Vm.b])
        xts, junk, hns, smalls = norm_scratch(sb)
        hnTs = [sb(f"hnT{i}", [128, 8, 512], BF16) for i in range(2)]
        qTs = [sb(f"qT{i}", [128, 4, 512], BF16) for i in range(2)]
        PTs = [sb(f"PT{i}", [128, 2, 512], BF16) for i in range(2)]
        obs = [sb(f"ob{i}", [128, 4, 512], BF16) for i in range(2)]
        recs = [sb(f"rec{i}", [128, 1], F32) for i in range(2)]
        P.barrier()
        cstop = C.cfg.get("cstop", 9)
        gi = pi = ri = 0
        for b in range(NSEQ):
            if cstop <= 1:
                break
            for g in range(NG):
                hnT, qT, ob = hnTs[gi % 2], qTs[gi % 2], obs[gi % 2]
                gi += 1
                tg = b * S + g * 512
                for i in range(4):
                    t0 = tg + i * 128
                    norm_transpose_tile(C, C.hcur[t0:t0 + 128, :], xts[i % 2], junk, smalls[i % 2], hns[i % 2], gain, hnT, i * 128)
                for hh in range(4):
                    bank = next_bank(C)
                    mm_group(C, bank[:], bank, [(wq[:, k, hh * 128:(hh + 1) * 128], hnT[:, k, :]) for k in range(8)], [wq.b, hnT.b])
                    CP(C, "act" if hh % 2 == 0 else "dve", qT[:, hh, :], bank[:], [bank.b], [qT.b])
                for hh in range(4):
                    if cstop <= 2:
                        break
                    PT = PTs[pi % 2]
                    pi += 1
                    for mb in range(2):
                        bank = next_bank(C)
                        MM(C, bank[:], KmT[:, b, hh, mb * 128:(mb + 1) * 128], qT[:, hh, :], True, True, [KmT.b, qT.b], [bank.b])
                        ACT(C, PT[:, mb, :], bank[:], AF.Exp, [bank.b], [PT.b])
                    for half in range(2):
                        if cstop <= 3:
                            break
                        bank = next_bank(C)
                        for ii in range(2):
                            i = half * 2 + ii
                            o = bank[:, ii * 256:ii * 256 + 129]
                            for mb in range(2):
                                MM(C, o, PT[:, mb, i * 128:(i + 1) * 128], Vm[:, b, mb, hh, :], mb == 0, mb == 1, [PT.b, Vm.b], [bank.b],
                                   signal=(mb == 1 and ii == 1))
                        for ii in range(2):
                            i = half * 2 + ii
                            rec = recs[ri % 2]
                            ri += 1
                            RCP(C, rec[:], bank[:, ii * 256 + 128:ii * 256 + 129], [bank.b], [rec.b])
                            TS(C, ob[:, i, hh * 128:(hh + 1) * 128], bank[:, ii * 256:ii * 256 + 128], rec[:], None, ALU.mult, None,
                               [bank.b, rec.b], [ob.b])
                DMA(C, D_["ymix"][tg:tg + 512, 0:512].rearrange("(i p) f -> p i f", p=128), ob[:], reads=[ob.b])
        P.barrier()
    if C.cfg.get("cstop", 9) <= 4:
        return
    dst = hnext(C)
    proj_residual(C, l, "C3", lambda i: D_["ymix"][i * 128:(i + 1) * 128, 0:512], 4, I["w_xo"][l], C.hcur, dst, C.T // 128)
    C.hcur = dst


BIG = 10000.0


def phase_D(C, l, last):
    nc, P, I, D_ = C.nc, C.P, C.I, C.D
    S, NSEQ, NG, NT = C.S, C.NSEQ, C.NG, C.NT
    if C.cfg.get("nofinal"):
        last = False
    with contextlib.ExitStack() as ph:
        def sb(name, shape, dt):
            return Tile(ph.enter_context(nc.sbuf_tensor(f"D{l}_" + name, shape, dt)), name)
        gain = sb("gain", [128, DM], F32)
        DMA(C, gain[:], I["moe_norm"][l].partition_broadcast(128), writes=[gain.b])
        Wr = sb("Wr", [128, 8, 20], F32)
        DMA(C, Wr[:, :, 0:4], I["w_rg"][l].rearrange("(k p) f -> p k f", p=128), writes=[Wr.b], allow_slow_non_contiguous=True)
        DMA(C, Wr[:, :, 4:20], I["w_re"][l].rearrange("(k p) f -> p k f", p=128), writes=[Wr.b], allow_slow_non_contiguous=True)
        Wrh = sb("Wrh", [128, 8, 20], BF16)
        Wrl = sb("Wrl", [128, 8, 20], BF16)
        CP(C, "dve", Wrh[:], Wr[:], [Wr.b], [Wrh.b])
        TT(C, Wrl[:], Wr[:], Wrh[:], ALU.subtract, [Wr.b, Wrh.b], [Wrl.b])
        brb = sb("brb", [128, 20], F32)
        DMA(C, brb[:, 0:4], I["b_rg"][l].partition_broadcast(128), writes=[brb.b])
        DMA(C, brb[:, 4:20], I["b_re"][l].partition_broadcast(128), writes=[brb.b])
        hnT = sb("hnT", [128, 8, S], BF16)
        acc = sb("acc", [128, NT, DM], F32)
        lg = sb("lg", [128, NT, 20], F32)
        G = sb("G", [128, NT, 16], F32)
        if last:
            fgain = sb("fgain", [128, DM], F32)
            DMA(C, fgain[:], I["final_norm"].partition_broadcast(128), writes=[fgain.b])
        P.barrier()
        hdst = hnext(C)
        for b in range(NSEQ):
            with contextlib.ExitStack() as ph2:
                def sb2(name, shape, dt):
                    return Tile(ph2.enter_context(nc.sbuf_tensor(f"D{l}r{b}_" + name, shape, dt)), name)
                xts = [sb2(f"xt{i}", [128, DM], F32) for i in range(2)]
                junk = sb2("junk", [128, DM], BF16)
                hn32s = [sb2(f"hn32{i}", [128, DM], F32) for i in range(2)]
                his = [sb2(f"hi{i}", [128, DM], BF16) for i in range(2)]
                los = [sb2(f"lo{i}", [128, DM], BF16) for i in range(2)]
                loTs = [sb2(f"loT{i}", [128, 8, 128], BF16) for i in range(2)]
                smalls = [[sb2(f"{n}{i}", [128, 1], F32) for n in ("ss", "ln", "rstd")] for i in range(2)]
                sm = {n: sb2(n, [128, NT], F32) for n in ("gmax", "sg", "pg", "m1", "m2", "den", "coef")}
                g4 = {n: sb2(n, [128, NT, 4], F32) for n in ("gm", "eg", "pen")}
                e16 = {n: sb2(n, [128, NT, 16], F32) for n in ("lem", "isf", "le2", "sel", "w")}
                P.barrier()
                for i in range(NT):
                    t0 = b * S + i * 128
                    xt, hn32, hi_, lo_, loT = xts[i % 2], hn32s[i % 2], his[i % 2], los[i % 2], loTs[i % 2]
                    norm_tile(C, C.hcur[t0:t0 + 128, :], xt, junk, smalls[i % 2], gain, hn32[:], hn32.b)
                    CP(C, "act", hi_[:], hn32[:], [hn32.b], [hi_.b])
                    TT(C, lo_[:], hn32[:], hi_[:], ALU.subtract, [hn32.b, hi_.b], [lo_.b])
                    tsl = slice(i * 128, (i + 1) * 128)
                    bank = next_bank(C)
                    bv = bf16view(bank)
                    for k in range(8):
                        TR(C, bv[:, k * 128:(k + 1) * 128], hi_[:, k * 128:(k + 1) * 128], C.idb[:], [hi_.b, C.idb.b], [bank.b], signal=(k == 7))
                    CP(C, "act", hnT[:, :, tsl], bv.rearrange("p (k t) -> p k t", k=8), [bank.b], [hnT.b])
                    bank = next_bank(C)
                    bv = bf16view(bank)
                    for k in range(8):
                        TR(C, bv[:, k * 128:(k + 1) * 128], lo_[:, k * 128:(k + 1) * 128], C.idb[:], [lo_.b, C.idb.b], [bank.b], signal=(k == 7))
                    CP(C, "dve", loT[:], bv.rearrange("p (k t) -> p k t", k=8), [bank.b], [loT.b])
                    bank = next_bank(C)
                    pairs = []
                    for k in range(8):
                        pairs += [(hnT[:, k, tsl], Wrh[:, k, :]), (loT[:, k, :], Wrh[:, k, :]), (hnT[:, k, tsl], Wrl[:, k, :])]
                    mm_group(C, bank[:, 0:20], bank, pairs, [hnT.b, loT.b, Wrh.b, Wrl.b])
                    TT(C, lg[:, i, :], bank[:, 0:20], brb[:], ALU.add, [bank.b, brb.b], [lg.b])
                gl = lg[:, :, 0:4]
                el = lg[:, :, 4:20]
                bc4 = lambda t_: t_[:].unsqueeze(2).to_broadcast([128, NT, 4])
                bc16 = lambda t_: t_[:].unsqueeze(2).to_broadcast([128, NT, 16])
                RED(C, sm["gmax"][:], gl, ALU.max, [lg.b], [sm["gmax"].b])
                TT(C, g4["gm"][:], gl, bc4(sm["gmax"]), ALU.is_ge, [lg.b, sm["gmax"].b], [g4["gm"].b])
                TT(C, g4["eg"][:], gl, bc4(sm["gmax"]), ALU.subtract, [lg.b, sm["gmax"].b], [g4["eg"].b])
                ACT(C, g4["eg"][:], g4["eg"][:], AF.Exp, [g4["eg"].b], [g4["eg"].b])
                RED(C, sm["sg"][:], g4["eg"][:], ALU.add, [g4["eg"].b], [sm["sg"].b])
                RCP(C, sm["pg"][:], sm["sg"][:], [sm["sg"].b], [sm["pg"].b])
                TS(C, g4["pen"][:], g4["gm"][:], BIG, -BIG, ALU.mult, ALU.add, [g4["gm"].b], [g4["pen"].b])
                for gq_ in range(4):
                    TT(C, e16["lem"][:, :, gq_ * 4:(gq_ + 1) * 4], lg[:, :, 4 + gq_ * 4:8 + gq_ * 4],
                       g4["pen"][:, :, gq_:gq_ + 1].to_broadcast([128, NT, 4]), ALU.add, [lg.b, g4["pen"].b], [e16["lem"].b])
                RED(C, sm["m1"][:], e16["lem"][:], ALU.max, [e16["lem"].b], [sm["m1"].b])
                TT(C, e16["isf"][:], e16["lem"][:], bc16(sm["m1"]), ALU.is_ge, [e16["lem"].b, sm["m1"].b], [e16["isf"].b])
                STT(C, e16["le2"][:], e16["isf"][:], -BIG, e16["lem"][:], ALU.mult, ALU.add, [e16["isf"].b, e16["lem"].b], [e16["le2"].b])
                RED(C, sm["m2"][:], e16["le2"][:], ALU.max, [e16["le2"].b], [sm["m2"].b])
                TT(C, e16["sel"][:], e16["lem"][:], bc16(sm["m2"]), ALU.is_ge, [e16["lem"].b, sm["m2"].b], [e16["sel"].b])
                TT(C, e16["w"][:], e16["lem"][:], bc16(sm["m1"]), ALU.subtract, [e16["lem"].b, sm["m1"].b], [e16["w"].b])
                ACT(C, e16["w"][:], e16["w"][:], AF.Exp, [e16["w"].b], [e16["w"].b])
                TT(C, e16["w"][:], e16["w"][:], e16["sel"][:], ALU.mult, [e16["w"].b, e16["sel"].b], [e16["w"].b])
                RED(C, sm["den"][:], e16["w"][:], ALU.add, [e16["w"].b], [sm["den"].b])
                RCP(C, sm["den"][:], sm["den"][:], [sm["den"].b], [sm["den"].b])
                TT(C, sm["coef"][:], sm["den"][:], sm["pg"][:], ALU.mult, [sm["den"].b, sm["pg"].b], [sm["coef"].b])
                TT(C, G[:], e16["w"][:], bc16(sm["coef"]), ALU.mult, [e16["w"].b, sm["coef"].b], [G.b])
                P.barrier()
            if C.cfg.get("dstop", 9) <= 1:
                continue
            with contextlib.ExitStack() as ph3:
                def sb3(name, shape, dt):
                    return Tile(ph3.enter_context(nc.sbuf_tensor(f"D{l}e{b}_" + name, shape, dt)), name)
                wst = [sb3(f"wst{i}", [128, 2048], F32) for i in range(2)]
                Wgs = [sb3(f"Wg{i}", [128, 8, DEXP], BF16) for i in range(2)]
                Wus = [sb3(f"Wu{i}", [128, 8, DEXP], BF16) for i in range(2)]
                Wds = [sb3(f"Wd{i}", [128, 4, DM], BF16) for i in range(2)]
                uTs = [sb3(f"uT{i}", [128, 4, 512], BF16) for i in range(2)]
                sgs = [sb3(f"sg{i}", [128, 512], BF16) for i in range(2)]
                P.barrier()
                ui = 0
                for e in range(NEXP):
                    Wg, Wu, Wd = Wgs[e % 2], Wus[e % 2], Wds[e % 2]
                    sg_ = I["w_eg"][l, e].rearrange("(k p) f -> p k f", p=128)
                    su_ = I["w_eu"][l, e].rearrange("(k p) f -> p k f", p=128)
                    sd_ = I["w_ed"][l, e].rearrange("(k p) f -> p k f", p=128)
                    load_weight_bf16(C, Wg, [Wg[:, 0:4, :], Wg[:, 4:8, :]], [sg_[:, 0:4, :], sg_[:, 4:8, :]], wst)
                    load_weight_bf16(C, Wu, [Wu[:, 0:4, :], Wu[:, 4:8, :]], [su_[:, 0:4, :], su_[:, 4:8, :]], wst)
                    load_weight_bf16(C, Wd, [Wd[:, 0:2, :], Wd[:, 2:4, :]], [sd_[:, 0:2, :], sd_[:, 2:4, :]], wst)
                    for g in range(NG):
                        uT = uTs[ui % 2]
                        ui += 1
                        xs = hnT[:, :, g * 512:(g + 1) * 512]
                        for fc in range(4):
                            sg = sgs[fc % 2]
                            bg = next_bank(C)
                            mm_group(C, bg[:], bg, [(Wg[:, k, fc * 128:(fc + 1) * 128], hnT[:, k, g * 512:(g + 1) * 512]) for k in range(8)],
                                     [Wg.b, hnT.b])
                            ACT(C, sg[:], bg[:], AF.Silu, [bg.b], [sg.b])
                            bu = next_bank(C)
                            mm_group(C, bu[:], bu, [(Wu[:, k, fc * 128:(fc + 1) * 128], hnT[:, k, g * 512:(g + 1) * 512]) for k in range(8)],
                                     [Wu.b, hnT.b])
                            TT(C, uT[:, fc, :], bu[:], sg[:], ALU.mult, [bu.b, sg.b], [uT.b])
                        for i in range(4):
                            ti = g * 4 + i
                            for n in range(2):
                                by = next_bank(C)
                                mm_group(C, by[:], by, [(uT[:, fc, i * 128:(i + 1) * 128], Wd[:, fc, n * 512:(n + 1) * 512]) for fc in range(4)],
                                         [uT.b, Wd.b])
                                a = acc[:, ti, n * 512:(n + 1) * 512]
                                if e == 0:
                                    TS(C, a, by[:], G[:, ti, e:e + 1], None, ALU.mult, None, [by.b, G.b], [acc.b])
                                else:
                                    STT(C, a, by[:], G[:, ti, e:e + 1], a, ALU.mult, ALU.add, [by.b, G.b, acc.b], [acc.b])
                P.barrier()
            if C.cfg.get("dstop", 9) <= 2:
                continue
            with contextlib.ExitStack() as ph4:
                def sb4(name, shape, dt):
                    return Tile(ph4.enter_context(nc.sbuf_tensor(f"D{l}f{b}_" + name, shape, dt)), name)
                hts = [sb4(f"ht{i}", [128, DM], F32) for i in range(2)]
                ots = [sb4(f"ot{i}", [128, DM], F32) for i in range(2)]
                junk = sb4("junk", [128, DM], BF16)
                smalls = [[sb4(f"{n}{i}", [128, 1], F32) for n in ("ss", "ln", "rstd")] for i in range(2)]
                P.barrier()
                for i in range(NT):
                    t0 = b * S + i * 128
                    ht = hts[i % 2]
                    ss, ln, rstd = smalls[i % 2]
                    DMA(C, ht[:], C.hcur[t0:t0 + 128, :], writes=[ht.b])
                    TT(C, ht[:], ht[:], acc[:, i, :], ALU.add, [ht.b, acc.b], [ht.b])
                    if not last:
                        DMA(C, hdst[t0:t0 + 128, :], ht[:], reads=[ht.b])
                    else:
                        ACT(C, junk[:], ht[:], AF.Square, [ht.b], [junk.b, ss.b], accum_out=ss[:])
                        rstd_ops(C, (ss[:], ss.b), (ln[:], ln.b), (rstd[:], rstd.b), 1.0 / DM)
                        ot = ots[i % 2]
                        STT(C, ot[:], ht[:], rstd[:], fgain[:], ALU.mult, ALU.mult, [ht.b, rstd.b, fgain.b], [ot.b])
                        odst = hdst if C.cfg.get("out_to_h") else C.out
                        DMA(C, odst[t0:t0 + 128, :], ot[:], reads=[ot.b])
                P.barrier()
        P.barrier()
        C.hcur = hdst


def consts():
    s = np.arange(128)[:, None]
    t = np.arange(128)[None, :]
    mq = np.zeros((128, 4, 512), np.float32)
    for r in range(4):
        for cb in range(4):
            blk = mq[:, r, cb * 128:(cb + 1) * 128]
            if cb < r:
                blk[:] = NEG
            elif cb == r:
                blk[:] = np.where(s <= t, 0.0, NEG)
    return {
        "c_maskq": mq.astype(ml_dtypes.bfloat16),
        "c_ident": np.eye(128, dtype=np.float32),
        "c_maskadd": np.where(s <= t, 0.0, NEG).astype(np.float32),
        "c_mask01": (s <= t).astype(np.float32),
    }


_RENAME = {"w_rg": "w_router_group", "b_rg": "b_router_group", "w_re": "w_router_expert", "b_re": "b_router_expert",
           "w_eg": "w_expert_gate", "w_eu": "w_expert_up", "w_ed": "w_expert_down"}
N_CORES = 8


def kernel(**inputs):
    x = np.asarray(inputs["x"], dtype=np.float32)
    mem = np.asarray(inputs["mem"], dtype=np.float32)
    B, S, _ = x.shape
    L = int(np.asarray(inputs["w_in"]).shape[0])
    NSEQ = B // N_CORES
    nc, C = build(dict(S=S, NSEQ=NSEQ, L=L, phases="0ABCD", debug=False))
    shared = {k: np.ascontiguousarray(np.asarray(v, dtype=np.float32)) for k, v in inputs.items() if k not in ("x", "mem")}
    shared.update(consts())
    in_maps = []
    for c in range(N_CORES):
        m = dict(shared)
        m["x"] = np.ascontiguousarray(x[c * NSEQ:(c + 1) * NSEQ].reshape(NSEQ * S, DM))
        m["mem"] = np.ascontiguousarray(mem[c * NSEQ:(c + 1) * NSEQ].reshape(NSEQ * MEM, DM))
        in_maps.append(m)
    res = run_bass_kernel_spmd(nc, in_maps, core_ids=list(range(N_CORES)))
    outs = [np.asarray(r["y_out"]).reshape(NSEQ, S, DM) for r in res.results]
    return np.concatenate(outs, axis=0).astype(np.float32)
```

```python
import contextlib
import numpy as np
import ml_dtypes
import concourse.bass as bass
import concourse.mybir as mybir
from concourse.bass_utils import run_bass_kernel_spmd

F32 = mybir.dt.float32
BF16 = mybir.dt.bfloat16
AF = mybir.ActivationFunctionType
ALU = mybir.AluOpType
AX = mybir.AxisListType

SAME_RAW = True
SEM_ROT = 30000

DM = 1024
MEM = 256
EPS = 1e-6
O_FQ, O_FK, O_FV, O_FF, O_GQ, O_GK, O_GV, O_GG, O_GA, O_END = 0, 512, 1024, 1536, 1544, 1800, 2056, 2568, 3080, 3096
NEXP = 16
DEXP = 512
NEG = -30000.0


class Buf:
    __slots__ = ("name", "w", "r")

    def __init__(self, name=""):
        self.name = name
        self.w = None
        self.r = {}


class Tile:
    def __init__(self, t, name=""):
        self.t = t
        self.b = Buf(name)

    def __getitem__(self, k):
        return self.t[k]


class _Queue:
    def __init__(self, name):
        self.name = name
        self.items = []
        self.sem = None
        self.cnt = 0
        self.semidx = 0
        self.seen = {}
        self.pending = False


class Prog:
    def __init__(self, nc, stack, ndma=8):
        self.nc = nc
        self.stack = stack
        self.q = {n: _Queue(n) for n in ("pe", "act", "dve", "pool", "sp")}
        for q in self.q.values():
            self._newsem(q)
        self.dsem = {}
        for qn in ("sp", "pool", "act"):
            sems = [stack.enter_context(nc.semaphore(f"d_{qn}_{i}")) for i in range(ndma)]
            self.dsem[qn] = {"sems": sems, "cnt": [0] * ndma, "idx": 0}

    def _newsem(self, q):
        q.sem = self.stack.enter_context(self.nc.semaphore(f"s_{q.name}_{q.semidx}"))
        q.semidx += 1
        q.cnt = 0

    def _deps(self, q, qn, reads, writes, is_dma):
        toks = []
        for b in reads:
            if b.w is not None:
                toks.append((b.w, "raw"))
        for b in writes:
            if b.w is not None:
                toks.append((b.w, "waw"))
            for t in b.r.values():
                toks.append((t, "war"))
        waits = []
        for (sem, cnt, src), kind in toks:
            if (not is_dma) and src == qn:
                if qn == "pe":
                    continue
                if not SAME_RAW:
                    continue
            if q.seen.get(sem, 0) >= cnt:
                continue
            q.seen[sem] = cnt
            waits.append((sem, cnt))
        return waits

    def _mark(self, tok, reads, writes):
        for b in writes:
            b.w = tok
            b.r = {}
        for b in reads:
            if b not in writes:
                b.r[tok[0]] = tok

    def op(self, qn, fn, reads=(), writes=(), signal=True):
        q = self.q[qn]
        waits = self._deps(q, qn, reads, writes, False)
        if signal:
            if q.cnt >= SEM_ROT and not q.pending:
                self._newsem(q)
            q.cnt += 1
            tok = (q.sem, q.cnt, qn)
            q.pending = False
            q.items.append((waits, fn, ("inc", q.sem)))
        else:
            tok = (q.sem, q.cnt + 1, qn)
            q.pending = True
            q.items.append((waits, fn, None))
        self._mark(tok, reads, writes)
        return tok

    def dma(self, qn, out, in_, reads=(), writes=(), **kw):
        q = self.q[qn]
        pool = self.dsem[qn]
        i = pool["idx"]
        pool["idx"] = (i + 1) % len(pool["sems"])
        sem = pool["sems"][i]
        prev = pool["cnt"][i]
        waits = self._deps(q, qn, reads, writes, True)
        if prev > 0 and q.seen.get(sem, 0) < prev:
            q.seen[sem] = prev
            waits.append((sem, prev))
        pool["cnt"][i] = prev + 16
        tok = (sem, prev + 16, qn + "_dma")
        q.items.append((waits, (lambda h: h.dma_start(out=out, in_=in_, **kw)), ("dma", sem)))
        self._mark(tok, reads, writes)
        return tok

    def barrier(self):
        toks = []
        for qn, q in self.q.items():
            if q.pending:
                raise RuntimeError("barrier with pending unsignaled op on " + qn)
            if q.cnt > 0:
                toks.append((q.sem, q.cnt, qn))
        for qn, pool in self.dsem.items():
            for s, c in zip(pool["sems"], pool["cnt"]):
                if c > 0:
                    toks.append((s, c, qn + "_dma"))
        for qn, q in self.q.items():
            waits = []
            for sem, cnt, src in toks:
                if src == qn:
                    continue
                if q.seen.get(sem, 0) >= cnt:
                    continue
                q.seen[sem] = cnt
                waits.append((sem, cnt))
            if waits:
                q.items.append((waits, None, None))

    def emit(self):
        self.barrier()
        nc = self.nc

        def runner(q):
            def f(h):
                for waits, fn, sig in q.items:
                    for sem, cnt in waits:
                        h.wait_ge(sem, cnt)
                    if fn is None:
                        continue
                    ins = fn(h)
                    if sig is None:
                        continue
                    if sig[0] == "dma":
                        ins.then_inc(sig[1], 16)
                    else:
                        ins.then_inc(sig[1], 1)
            return f

        with nc.Block() as block:
            block.tensor(runner(self.q["pe"]))
            block.scalar(runner(self.q["act"]))
            block.vector(runner(self.q["dve"]))
            block.gpsimd(runner(self.q["pool"]))
            block.sync(runner(self.q["sp"]))

    def stats(self):
        return {n: len(q.items) for n, q in self.q.items()}


class Ctx:
    pass


def build(cfg):
    S = cfg["S"]
    NSEQ = cfg["NSEQ"]
    L = cfg["L"]
    phases = cfg.get("phases", "ABCDE")
    debug = cfg.get("debug", False)
    T = S * NSEQ
    NT = S // 128
    NG = S // 512
    nc = bass.Bass("TRN2", target_bir_lowering=False)
    C = Ctx()
    C.nc, C.S, C.NSEQ, C.L, C.T, C.NT, C.NG = nc, S, NSEQ, L, T, NT, NG
    C.cfg = cfg

    def din(name, shape, dt=F32):
        return nc.dram_tensor(name, list(shape), dt, kind="ExternalInput").ap()

    def dscr(name, shape, dt):
        return nc.dram_tensor(name, list(shape), dt, kind=("ExternalOutput" if debug else "Internal")).ap()

    I = {}
    I["x"] = din("x", [T, DM])
    I["mem"] = din("mem", [NSEQ * MEM, DM])
    I["mem_norm"] = din("mem_norm", [DM])
    I["mix_norm"] = din("mix_norm", [L, DM])
    I["w_in"] = din("w_in", [L, DM, O_END])
    I["b_forget"] = din("b_forget", [L, 8])
    I["w_alpha_up"] = din("w_alpha_up", [L, 16, 256])
    I["b_alpha"] = din("b_alpha", [L, 256])
    I["fox_out_gain"] = din("fox_out_gain", [L, 512])
    I["gla_out_gain"] = din("gla_out_gain", [L, 512])
    I["w_out"] = din("w_out", [L, DM, DM])
    I["cross_norm"] = din("cross_norm", [L, DM])
    I["w_xq"] = din("w_xq", [L, DM, 512])
    I["w_xk"] = din("w_xk", [L, DM, 512])
    I["w_xv"] = din("w_xv", [L, DM, 512])
    I["w_xo"] = din("w_xo", [L, 512, DM])
    I["moe_norm"] = din("moe_norm", [L, DM])
    I["w_rg"] = din("w_router_group", [L, DM, 4])
    I["b_rg"] = din("b_router_group", [L, 4])
    I["w_re"] = din("w_router_expert", [L, DM, 16])
    I["b_re"] = din("b_router_expert", [L, 16])
    I["w_eg"] = din("w_expert_gate", [L, NEXP, DM, DEXP])
    I["w_eu"] = din("w_expert_up", [L, NEXP, DM, DEXP])
    I["w_ed"] = din("w_expert_down", [L, NEXP, DEXP, DM])
    I["final_norm"] = din("final_norm", [DM])
    I["c_ident"] = din("c_ident", [128, 128])
    I["c_maskadd"] = din("c_maskadd", [128, 128])
    I["c_mask01"] = din("c_mask01", [128, 128])
    I["c_maskq"] = din("c_maskq", [128, 4, 512], BF16)
    C.I = I
    out = nc.dram_tensor("y_out", [T, DM], F32, kind="ExternalOutput").ap()
    C.out = out

    D_ = {}
    D_["h"] = dscr("s_h", [T, DM], F32)
    D_["h2"] = dscr("s_h2", [T, DM], F32)
    C.hcur = I["x"]
    D_["QT"] = dscr("s_QT", [NSEQ, 4, 128, S], BF16)
    D_["KT"] = dscr("s_KT", [NSEQ, 4, 128, S], BF16)
    D_["vaug"] = dscr("s_vaug", [T, 520], BF16)
    D_["AB"] = dscr("s_AB", [NSEQ, 8, 6, S], BF16)
    D_["qgT"] = dscr("s_qgT", [NSEQ, 64, 4, S], BF16)
    D_["kgT"] = dscr("s_kgT", [NSEQ, 64, 4, S], BF16)
    D_["ktok"] = dscr("s_ktok", [T, 256], BF16)
    D_["gv"] = dscr("s_gv", [T, 512], BF16)
    D_["gg"] = dscr("s_gg", [T, 512], BF16)
    D_["ebl"] = dscr("s_ebl", [NSEQ, 64, 4, NT], F32)
    D_["ymix"] = dscr("s_ymix", [T, DM], BF16)
    C.D = D_

    with contextlib.ExitStack() as st:
        P = Prog(nc, st)
        C.P = P

        def gsb(name, shape, dt):
            return Tile(st.enter_context(nc.sbuf_tensor("g_" + name, shape, dt)), name)

        C.banks = [Tile(st.enter_context(nc.psum_tensor(f"bank{i}", [128, 512], F32)), f"bank{i}") for i in range(8)]
        C.bank_i = 0
        C.idf = gsb("idf", [128, 128], F32)
        C.idb = gsb("idb", [128, 128], BF16)
        C.maskadd = gsb("maskadd", [128, 128], BF16)
        C.mask01 = gsb("mask01", [128, 128], F32)
        C.ones = gsb("ones", [128, 512], F32)
        C.memnT = gsb("memnT", [128, 8, NSEQ * MEM], BF16)
        tmpc = gsb("tmpc", [128, 128], F32)
        P.dma("sp", C.idf[:], I["c_ident"], writes=[C.idf.b])
        P.op("act", lambda h: h.copy(out=C.idb[:], in_=C.idf[:]), reads=[C.idf.b], writes=[C.idb.b])
        P.dma("sp", tmpc[:], I["c_maskadd"], writes=[tmpc.b])
        P.op("act", lambda h: h.copy(out=C.maskadd[:], in_=tmpc[:]), reads=[tmpc.b], writes=[C.maskadd.b])
        P.dma("sp", C.mask01[:], I["c_mask01"], writes=[C.mask01.b])
        P.op("dve", lambda h: h.memset(C.ones[:], 1.0), writes=[C.ones.b])
        C.maskq = gsb("maskq", [128, 4, 512], BF16)
        P.dma("sp", C.maskq[:], I["c_maskq"], writes=[C.maskq.b])

        if "0" in phases:
            phase_mem(C)
        for l in range(L):
            if "A" in phases:
                phase_A(C, l)
            if "B" in phases or "1" in phases:
                phase_B1(C, l)
            if "B" in phases or "2" in phases:
                phase_B2(C, l)
            if "B" in phases or "3" in phases:
                phase_B3(C, l)
            if "C" in phases:
                phase_C(C, l)
            if "D" in phases:
                phase_D(C, l, last=(l == L - 1))
        P.emit()
        C.stats = P.stats()
    return nc, C


def next_bank(C):
    b = C.banks[C.bank_i]
    C.bank_i = (C.bank_i + 1) % 8
    return b


def bf16view(bank):
    return bank.t[:].bitcast(BF16)


def MM(C, out, lhsT, rhs, start, stop, reads, writes, signal=True):
    C.P.op("pe", lambda h: h.matmul(out=out, lhsT=lhsT, rhs=rhs, start=start, stop=stop), reads, writes, signal)


def TR(C, out, in_, ident, reads, writes, signal=True):
    C.P.op("pe", lambda h: h.transpose(out=out, in_=in_, identity=ident), reads, writes, signal)


def ACT(C, out, in_, func, reads, writes, **kw):
    C.P.op("act", lambda h: h.activation(out=out, in_=in_, func=func, **kw), reads, writes)


def TT(C, out, in0, in1, op, reads, writes):
    C.P.op("dve", lambda h: h.tensor_tensor(out=out, in0=in0, in1=in1, op=op), reads, writes)


def TS(C, out, in0, s1, s2, op0, op1, reads, writes):
    if s2 is None:
        C.P.op("dve", lambda h: h.tensor_scalar(out=out, in0=in0, scalar1=s1, scalar2=None, op0=op0), reads, writes)
    else:
        C.P.op("dve", lambda h: h.tensor_scalar(out=out, in0=in0, scalar1=s1, scalar2=s2, op0=op0, op1=op1), reads, writes)


def STT(C, out, in0, scalar, in1, op0, op1, reads, writes):
    C.P.op("dve", lambda h: h.scalar_tensor_tensor(out=out, in0=in0, scalar=scalar, in1=in1, op0=op0, op1=op1), reads, writes)


def CP(C, eng, out, in_, reads, writes, scale=None):
    if eng == "act":
        if scale is None:
            C.P.op("act", lambda h: h.copy(out=out, in_=in_), reads, writes)
        else:
            C.P.op("act", lambda h: h.mul(out=out, in_=in_, mul=scale), reads, writes)
    else:
        if scale is None:
            C.P.op("dve", lambda h: h.tensor_copy(out=out, in_=in_), reads, writes)
        else:
            TS(C, out, in_, scale, None, ALU.mult, None, reads, writes)


def MS(C, ap, val, writes):
    C.P.op("dve", lambda h: h.memset(ap, val), (), writes)


def RED(C, out, in_, op, reads, writes):
    C.P.op("dve", lambda h: h.tensor_reduce(out=out, in_=in_, axis=AX.X, op=op), reads, writes)


def RCP(C, out, in_, reads, writes):
    C.P.op("dve", lambda h: h.reciprocal(out=out, in_=in_), reads, writes)


def SCAN(C, out, d0, d1, init, reads, writes):
    C.P.op("dve", lambda h: h.tensor_tensor_scan(out=out, data0=d0, data1=d1, initial=init, op0=ALU.mult, op1=ALU.add), reads, writes)


def DMA(C, out, in_, reads=(), writes=(), **kw):
    C.P.dma("sp", out, in_, reads, writes, **kw)


def rstd_ops(C, ss, ln, rstd, inv_n):
    ACT(C, ln[0], ss[0], AF.Ln, [ss[1]], [ln[1]], scale=inv_n, bias=EPS)
    ACT(C, rstd[0], ln[0], AF.Exp, [ln[1]], [rstd[1]], scale=-0.5)


def mm_group(C, out_ap, bank, pairs, reads):
    n = len(pairs)
    for k, (a, b) in enumerate(pairs):
        MM(C, out_ap, a, b, k == 0, k == n - 1, reads, [bank.b], signal=(k == n - 1))


def load_weight_bf16(C, dst_tile, dst_aps, src_aps, stage_tiles):
    for k, (dap, src) in enumerate(zip(dst_aps, src_aps)):
        stg = stage_tiles[k % len(stage_tiles)]
        shp = list(src.shape)
        sap = stg.t[:]
        if list(sap.shape) != shp:
            if len(shp) == 2:
                sap = stg.t[0:shp[0], 0:shp[1]]
            else:
                sap = stg.t[0:shp[0], 0:shp[1] * shp[2]].rearrange("p (a b) -> p a b", a=shp[1])
        DMA(C, sap, src, writes=[stg.b])
        CP(C, "act", dap, sap, [stg.b], [dst_tile.b])


def norm_tile(C, src_dram_ap, xt, junk, small, gain_bc, out_ap, out_buf):
    ss, ln, rstd = small
    DMA(C, xt[:], src_dram_ap, writes=[xt.b])
    ACT(C, junk[:], xt[:], AF.Square, [xt.b], [junk.b, ss.b], accum_out=ss[:])
    rstd_ops(C, (ss[:], ss.b), (ln[:], ln.b), (rstd[:], rstd.b), 1.0 / DM)
    STT(C, out_ap, xt[:], rstd[:], gain_bc[:], ALU.mult, ALU.mult, [xt.b, rstd.b, gain_bc.b], [out_buf])


def norm_transpose_tile(C, src_dram_ap, xt, junk, small, hn, gain_bc, hnT, col0):
    norm_tile(C, src_dram_ap, xt, junk, small, gain_bc, hn[:], hn.b)
    bank = next_bank(C)
    bv = bf16view(bank)
    for k in range(8):
        TR(C, bv[:, k * 128:(k + 1) * 128], hn[:, k * 128:(k + 1) * 128], C.idb[:], [hn.b, C.idb.b], [bank.b], signal=(k == 7))
    CP(C, "dve", hnT[:, :, col0:col0 + 128], bv.rearrange("p (k t) -> p k t", k=8), [bank.b], [hnT.b])


def norm_scratch(sb):
    xts = [sb(f"xt{i}", [128, DM], F32) for i in range(2)]
    junk = sb("junk", [128, DM], BF16)
    hns = [sb(f"hn{i}", [128, DM], BF16) for i in range(2)]
    smalls = [[sb(f"{n}{i}", [128, 1], F32) for n in ("ss", "ln", "rstd")] for i in range(2)]
    return xts, junk, hns, smalls


def phase_mem(C):
    nc, P, I = C.nc, C.P, C.I
    with contextlib.ExitStack() as ph:
        def sb(name, shape, dt):
            return Tile(ph.enter_context(nc.sbuf_tensor("M_" + name, shape, dt)), name)
        gain = sb("gain", [128, DM], F32)
        DMA(C, gain[:], I["mem_norm"].partition_broadcast(128), writes=[gain.b])
        xts, junk, hns, smalls = norm_scratch(sb)
        for i in range(C.NSEQ * MEM // 128):
            norm_transpose_tile(C, I["mem"][i * 128:(i + 1) * 128, :], xts[i % 2], junk, smalls[i % 2], hns[i % 2],
                                gain, C.memnT, i * 128)
        P.barrier()


def phase_A(C, l):
    nc, P, I, D_ = C.nc, C.P, C.I, C.D
    S, NSEQ, NG, NT = C.S, C.NSEQ, C.NG, C.NT
    hsrc = C.hcur
    with contextlib.ExitStack() as ph:
        def sb(name, shape, dt):
            return Tile(ph.enter_context(nc.sbuf_tensor(f"A{l}_" + name, shape, dt)), name)
        win = sb("win", [128, 8, O_END], BF16)
        HW_ = O_END // 2
        wst = [sb(f"wst{i}", [128, HW_], F32) for i in range(2)]
        load_weight_bf16(C, win, [win[:, k // 2, (k % 2) * HW_:(k % 2 + 1) * HW_] for k in range(16)],
                         [I["w_in"][l, (k // 2) * 128:(k // 2 + 1) * 128, (k % 2) * HW_:(k % 2 + 1) * HW_] for k in range(16)], wst)
        wup = sb("wup", [16, 256], BF16)
        load_weight_bf16(C, wup, [wup[:]], [I["w_alpha_up"][l]], wst)
        gain = sb("gain", [128, DM], F32)
        DMA(C, gain[:], I["mix_norm"][l].partition_broadcast(128), writes=[gain.b])
        nba = sb("nba", [64, 4], F32)
        nbf = sb("nbf", [8, 1], F32)
        DMA(C, nba[:], I["b_alpha"][l].rearrange("(h d) -> d h", d=64), writes=[nba.b], allow_slow_non_contiguous=True)
        DMA(C, nbf[:], I["b_forget"][l].rearrange("(h o) -> h o", o=1), writes=[nbf.b])
        CP(C, "act", nba[:], nba[:], [nba.b], [nba.b], scale=-1.0)
        CP(C, "act", nbf[:], nbf[:], [nbf.b], [nbf.b], scale=-1.0)
        ones8 = sb("ones8", [8, 512], F32)
        MS(C, ones8[:], 1.0, [ones8.b])
        xts, junk, hns, smalls = norm_scratch(sb)
        hnTs = [sb(f"hnT{i}", [128, 8, 512], BF16) for i in range(2)]
        stqs = [sb(f"stq{i}", [128, 8, 512], BF16) for i in range(1)] * 2
        stvs = [sb(f"stv{i}", [128, 4, 8, 65], BF16) for i in range(2)]
        for t_ in stvs:
            MS(C, t_[:], 1.0, [t_.b])
        stgv = [sb(f"stgv{i}", [128, 4, 512], BF16) for i in range(1)] * 2
        stgg = [sb(f"stgg{i}", [128, 4, 512], BF16) for i in range(1)] * 2
        gaT = sb("gaT", [16, 512], BF16)
        e1 = sb("e1", [64, 4, 512], F32)
        LgG = sb("LgG", [64, 4, 513], F32)
        Dg = sb("Dg", [64, 4, 512], F32)
        expb = sb("expb", [64, 4, 512], F32)
        expnb = e1
        qts = [sb(f"qgt{i}", [64, 4, 512], BF16) for i in range(2)]
        kts = [sb(f"kgt{i}", [64, 4, 512], BF16) for i in range(2)]
        ktoks = [sb(f"ktok{i}", [128, 4, 256], BF16) for i in range(2)]
        ebl = sb("ebl", [64, 4, NT], F32)
        ef = sb("ef", [8, 512], F32)
        lf = ef
        Lf = sb("Lf", [8, 513], F32)
        r1 = sb("r1", [8, 512], F32)
        ABst = sb("ABst", [8, 6, 512], BF16)
        P.barrier()

        gi = 0
        for b in range(NSEQ):
            MS(C, LgG[:, :, 0:1], 0.0, [LgG.b])
            MS(C, Lf[:, 0:1], 0.0, [Lf.b])
            for g in range(NG):
                hnT, stq, stv, sgv, sgg = hnTs[gi % 2], stqs[gi % 2], stvs[gi % 2], stgv[gi % 2], stgg[gi % 2]
                qt, kt, ktok = qts[gi % 2], kts[gi % 2], ktoks[gi % 2]
                gi += 1
                tg = b * S + g * 512
                gsl = slice(g * 512, (g + 1) * 512)
                for i in range(4):
                    t0 = tg + i * 128
                    norm_transpose_tile(C, hsrc[t0:t0 + 128, :], xts[i % 2], junk, smalls[i % 2], hns[i % 2], gain, hnT, i * 128)
                for c in range(8):
                    col = (O_FQ if c < 4 else O_FK) + (c % 4) * 128
                    bank = next_bank(C)
                    mm_group(C, bank[:], bank, [(win[:, k, col:col + 128], hnT[:, k, :]) for k in range(8)], [win.b, hnT.b])
                    CP(C, "act" if c % 2 == 0 else "dve", stq[:, c, :], bank[:], [bank.b], [stq.b], scale=(0.125 if c < 4 else None))
                DMA(C, D_["QT"][b, :, :, gsl].rearrange("c p t -> p c t"), stq[:, 0:4, :], reads=[stq.b])
                DMA(C, D_["KT"][b, :, :, gsl].rearrange("c p t -> p c t"), stq[:, 4:8, :], reads=[stq.b])
                bank = next_bank(C)
                mm_group(C, bank[0:8, :], bank, [(win[:, k, O_FF:O_FF + 8], hnT[:, k, :]) for k in range(8)], [win.b, hnT.b])
                ACT(C, ef[:], bank[0:8, :], AF.Exp, [bank.b, nbf.b], [ef.b], scale=-1.0, bias=nbf[:])
                ACT(C, lf[:], ef[:], AF.Ln, [ef.b], [lf.b], bias=1.0)
                SCAN(C, Lf[:, 1:513], ones8[:], lf[:], Lf[:, 0:1], [ones8.b, lf.b, Lf.b], [Lf.b])
                CP(C, "dve", ABst[:, 3, :], Lf[:, 1:513], [Lf.b], [ABst.b])
                TT(C, r1[:], Lf[:, 1:513], ABst[:, 3, :], ALU.subtract, [Lf.b, ABst.b], [r1.b])
                CP(C, "dve", ABst[:, 4, :], r1[:], [r1.b], [ABst.b])
                TT(C, r1[:], r1[:], ABst[:, 4, :], ALU.subtract, [r1.b, ABst.b], [r1.b])
                CP(C, "dve", ABst[:, 5, :], r1[:], [r1.b], [ABst.b])
                CP(C, "act", ABst[:, 0:3, :], ABst[:, 3:6, :], [ABst.b], [ABst.b], scale=-1.0)
                DMA(C, D_["AB"][b, :, :, gsl], ABst[:], reads=[ABst.b])
                CP(C, "dve", Lf[:, 0:1], Lf[:, 512:513], [Lf.b], [Lf.b])
                bank = next_bank(C)
                mm_group(C, bank[0:16, :], bank, [(win[:, k, O_GA:O_GA + 16], hnT[:, k, :]) for k in range(8)], [win.b, hnT.b])
                CP(C, "act", gaT[:], bank[0:16, :], [bank.b], [gaT.b])
                for hh in range(4):
                    bank = next_bank(C)
                    mm_group(C, bank[0:64, :], bank, [(wup[:, hh * 64:(hh + 1) * 64], gaT[:])], [wup.b, gaT.b])
                    ACT(C, e1[:, hh, :], bank[0:64, :], AF.Exp, [bank.b, nba.b], [e1.b], scale=-1.0, bias=nba[:, hh:hh + 1])
                ACT(C, e1[:], e1[:], AF.Ln, [e1.b], [e1.b], bias=1.0)
                for hh in range(4):
                    SCAN(C, LgG[:, hh, 1:513], C.ones[0:64, :], e1[:, hh, :], LgG[:, hh, 0:1], [C.ones.b, e1.b, LgG.b], [LgG.b])
                TT(C, Dg[:].rearrange("p h (c t) -> p h c t", t=128),
                   LgG[:, :, 1:513].rearrange("p h (c t) -> p h c t", t=128),
                   LgG[:, :, 0:512].rearrange("p h (c t) -> p h c t", t=128)[:, :, :, 0:1].to_broadcast([64, 4, 4, 128]),
                   ALU.subtract, [LgG.b], [Dg.b])
                ACT(C, expb[:], Dg[:], AF.Exp, [Dg.b], [expb.b], scale=-1.0 / 16)
                ACT(C, expnb[:], Dg[:], AF.Exp, [Dg.b], [expnb.b], scale=1.0 / 16)
                CP(C, "dve", ebl[:, :, g * 4:(g + 1) * 4], expb[:].rearrange("p h (c t) -> p h c t", t=128)[:, :, :, 127], [expb.b], [ebl.b])
                CP(C, "dve", LgG[:, :, 0:1], LgG[:, :, 512:513], [LgG.b], [LgG.b])
                for hh in range(4):
                    bank = next_bank(C)
                    col = O_GQ + hh * 64
                    mm_group(C, bank[0:64, :], bank, [(win[:, k, col:col + 64], hnT[:, k, :]) for k in range(8)], [win.b, hnT.b])
                    STT(C, qt[:, hh, :], bank[0:64, :], 0.125, expb[:, hh, :], ALU.mult, ALU.mult, [bank.b, expb.b], [qt.b])
                    bank = next_bank(C)
                    col = O_GK + hh * 64
                    mm_group(C, bank[0:64, :], bank, [(win[:, k, col:col + 64], hnT[:, k, :]) for k in range(8)], [win.b, hnT.b])
                    TT(C, kt[:, hh, :], bank[0:64, :], expnb[:, hh, :], ALU.mult, [bank.b, expnb.b], [kt.b])
                DMA(C, D_["qgT"][b, :, :, gsl], qt[:], reads=[qt.b])
                DMA(C, D_["kgT"][b, :, :, gsl], kt[:], reads=[kt.b])
                bank = next_bank(C)
                bv = bf16view(bank)
                for c in range(4):
                    for hh in range(4):
                        o0 = c * 256 + hh * 64
                        TR(C, bv[:, o0:o0 + 64], kt[:, hh, c * 128:(c + 1) * 128], C.idb[0:64, 0:64], [kt.b, C.idb.b], [bank.b],
                           signal=(c == 3 and hh == 3))
                CP(C, "act", ktok[:], bv.rearrange("p (c f) -> p c f", c=4), [bank.b], [ktok.b])
                DMA(C, D_["ktok"][tg:tg + 512, :].rearrange("(c p) f -> p c f", p=128), ktok[:], reads=[ktok.b])
                for i in range(4):
                    lhs = [hnT[:, k, i * 128:(i + 1) * 128] for k in range(8)]
                    bank = next_bank(C)
                    mm_group(C, bank[:], bank, [(lhs[k], win[:, k, O_FV:O_FV + 512]) for k in range(8)], [win.b, hnT.b])
                    CP(C, "dve", stv[:, i, :, 0:64], bank[:].rearrange("p (h d) -> p h d", d=64), [bank.b], [stv.b])
                    bank = next_bank(C)
                    mm_group(C, bank[:], bank, [(lhs[k], win[:, k, O_GV:O_GV + 512]) for k in range(8)], [win.b, hnT.b])
                    CP(C, "act", sgv[:, i, :], bank[:], [bank.b], [sgv.b])
                    bank = next_bank(C)
                    mm_group(C, bank[:], bank, [(lhs[k], win[:, k, O_GG:O_GG + 512]) for k in range(8)], [win.b, hnT.b])
                    ACT(C, sgg[:, i, :], bank[:], AF.Silu, [bank.b], [sgg.b])
                DMA(C, D_["vaug"][tg:tg + 512, :].rearrange("(i p) f -> p i f", p=128), stv[:].rearrange("p i h d -> p i (h d)"), reads=[stv.b])
                DMA(C, D_["gv"][tg:tg + 512, :].rearrange("(i p) f -> p i f", p=128), sgv[:], reads=[sgv.b])
                DMA(C, D_["gg"][tg:tg + 512, :].rearrange("(i p) f -> p i f", p=128), sgg[:], reads=[sgg.b])
            DMA(C, D_["ebl"][b], ebl[:], reads=[ebl.b])
        P.barrier()


def phase_B1(C, l):
    nc, P, I, D_ = C.nc, C.P, C.I, C.D
    S, NSEQ, NT = C.S, C.NSEQ, C.NT
    NJ = NT // 4
    with contextlib.ExitStack() as ph:
        def sb(name, shape, dt):
            return Tile(ph.enter_context(nc.sbuf_tensor(f"B1{l}_" + name, shape, dt)), name)
        QT = sb("QT", [128, 4, S], BF16)
        KT = sb("KT", [128, 4, S], BF16)
        Va = sb("Va", [128, NT, 520], BF16)
        Ars = [sb(f"Ar{i}", [128, S], BF16) for i in range(2)]
        Brs = [sb(f"Br{i}", [128, S], BF16) for i in range(2)]
        for t_ in Ars + Brs:
            MS(C, t_[:], 0.0, [t_.b])
        for k_ in range(2):
            MS(C, Ars[k_][k_ * 64:k_ * 64 + 6, :], 1.0, [Ars[k_].b])
            MS(C, Brs[k_][k_ * 64:k_ * 64 + 6, :], 1.0, [Brs[k_].b])
        PTs = [sb(f"PT{i}", [128, 512], BF16) for i in range(4)]
        oTs = [sb(f"oT{i}", [65, 512], BF16) for i in range(2)]
        rec4s = [sb(f"rec4{i}", [128, 4], F32) for i in range(2)]
        fo = sb("fo", [128, NT, 512], F32)
        gain = sb("gain", [128, 512], F32)
        DMA(C, gain[:], I["fox_out_gain"][l].partition_broadcast(128), writes=[gain.b])
        tmpN = sb("tmpN", [128, NT, 512], F32)
        ssqN = sb("ssqN", [128, NT * 8], F32)
        lnqN = sb("lnqN", [128, NT * 8], F32)
        rsqN = sb("rsqN", [128, NT * 8], F32)
        ybs = [sb(f"yb{i}", [128, 512], BF16) for i in range(4)]
        P.barrier()
        sbanks = C.banks[0:4]
        abanks = C.banks[4:6]
        tbanks = C.banks[6:8]
        si = ai = hi = pi = 0
        for b in range(NSEQ):
            DMA(C, QT[:], D_["QT"][b].rearrange("c p t -> p c t"), writes=[QT.b])
            DMA(C, KT[:], D_["KT"][b].rearrange("c p t -> p c t"), writes=[KT.b])
            DMA(C, Va[:], D_["vaug"][b * S:(b + 1) * S, :].rearrange("(i p) f -> p i f", p=128), writes=[Va.b])
            for hh in range(8):
                c = hh // 2
                pr = (hh % 2) * 64
                Ar, Br = Ars[hh % 2], Brs[hh % 2]
                DMA(C, Ar[pr + 3:pr + 6, :], D_["AB"][b, hh, 3:6, :], writes=[Ar.b])
                DMA(C, Br[pr:pr + 3, :], D_["AB"][b, hh, 0:3, :], writes=[Br.b])
                for J in range(NJ):
                    tq = slice(J * 512, (J + 1) * 512)
                    nI = 4 * J + 4
                    ab = abanks[ai % 2]
                    oT = oTs[ai % 2]
                    rec4 = rec4s[ai % 2]
                    tb = tbanks[ai % 2]
                    ai += 1
                    pend = None
                    for i in range(nI + 1):
                        if i < nI:
                            ks = slice(i * 128, (i + 1) * 128)
                            bank = sbanks[si % 4]
                            si += 1
                            PT = PTs[pi % 4]
                            pi += 1
                            diag = (i >= 4 * J)
                            MM(C, bank[:], KT[pr:pr + 64, c, ks], QT[pr:pr + 64, c, tq], True, False, [KT.b, QT.b], [bank.b], signal=False)
                            MM(C, bank[:], Ar[pr:pr + 64, ks], Br[pr:pr + 64, tq], False, not diag, [Ar.b, Br.b], [bank.b], signal=(not diag))
                            if diag:
                                MM(C, bank[:], C.idb[:], C.maskq[:, i - 4 * J, :], False, True, [C.idb.b, C.maskq.b], [bank.b], signal=True)
                            ACT(C, PT[:], bank[:], AF.Exp, [bank.b], [PT.b])
                        if pend is not None:
                            ip, PTp = pend
                            MM(C, ab[0:65, :], Va[:, ip, hh * 65:(hh + 1) * 65], PTp[:], ip == 0, ip == nI - 1, [Va.b, PTp.b], [ab.b],
                               signal=(ip == nI - 1))
                        pend = (i, PT) if i < nI else None
                    CP(C, "act", oT[:], ab[0:65, :], [ab.b], [oT.b])
                    tbv = bf16view(tb)
                    for jj in range(4):
                        TR(C, tbv[:, jj * 128:jj * 128 + 65], oT[:, jj * 128:(jj + 1) * 128], C.idb[0:65, 0:65], [oT.b, C.idb.b], [tb.b],
                           signal=(jj == 3))
                    tb3 = tbv[:, 0:512].rearrange("p (j f) -> p j f", f=128)
                    RCP(C, rec4[:], tb3[:, :, 64], [tb.b], [rec4.b])
                    TT(C, fo[:, 4 * J:4 * J + 4, hh * 64:(hh + 1) * 64], tb3[:, :, 0:64], rec4[:].unsqueeze(2).to_broadcast([128, 4, 64]), ALU.mult,
                       [tb.b, rec4.b], [fo.b])
            fo2 = fo[:].rearrange("p n f -> p (n f)")
            tm2 = tmpN[:].rearrange("p n f -> p (n f)")
            TT(C, tm2, fo2, fo2, ALU.mult, [fo.b], [tmpN.b])
            RED(C, ssqN[:], tm2.rearrange("p (m d) -> p m d", d=64), ALU.add, [tmpN.b], [ssqN.b])
            rstd_ops(C, (ssqN[:], ssqN.b), (lnqN[:], lnqN.b), (rsqN[:], rsqN.b), 1.0 / 64)
            TT(C, tm2.rearrange("p (m d) -> p m d", d=64), fo2.rearrange("p (m d) -> p m d", d=64),
               rsqN[:].unsqueeze(2).to_broadcast([128, NT * 8, 64]), ALU.mult, [fo.b, rsqN.b], [tmpN.b])
            for j in range(NT):
                yb = ybs[j % 4]
                TT(C, yb[:], tmpN[:, j, :], gain[:], ALU.mult, [tmpN.b, gain.b], [yb.b])
                r0 = b * S + j * 128
                DMA(C, D_["ymix"][r0:r0 + 128, 0:512], yb[:], reads=[yb.b])
        P.barrier()


def phase_B2(C, l):
    nc, P, I, D_ = C.nc, C.P, C.I, C.D
    S, NSEQ, NT = C.S, C.NSEQ, C.NT
    with contextlib.ExitStack() as ph:
        def sb(name, shape, dt):
            return Tile(ph.enter_context(nc.sbuf_tensor(f"B2{l}_" + name, shape, dt)), name)
        qg = sb("qg", [64, 4, S], BF16)
        kg = sb("kg", [64, 4, S], BF16)
        ktok = sb("ktok", [128, NT, 256], BF16)
        gv = sb("gv", [128, NT, 512], BF16)
        gg = sb("gg", [128, NT, 512], BF16)
        ebl = sb("ebl", [64, 4, NT], F32)
        Sf = sb("Sf", [64, 4, 128], F32)
        Sb_ = sb("Sb", [64, 4, 128], BF16)
        tS = sb("tS", [64, 4, 128], F32)
        atts = [sb(f"att{i}", [128, 4, 128], BF16) for i in range(2)]
        gain = sb("gain", [128, 512], F32)
        DMA(C, gain[:], I["gla_out_gain"][l].partition_broadcast(128), writes=[gain.b])
        sq = sb("sq", [128, 512], F32)
        ssq = sb("ssq", [128, 4], F32)
        lnq = sb("lnq", [128, 4], F32)
        rsq = sb("rsq", [128, 4], F32)
        t1 = sb("t1", [128, 512], F32)
        ybs = [sb(f"yb{i}", [128, 512], BF16) for i in range(3)]
        deferred = []

        def gla_norm(bk_o, yb, c, b_=None):
            ACT(C, sq[:], bk_o[:], AF.Square, [bk_o.b], [sq.b])
            RED(C, ssq[:], sq[:].rearrange("p (h d) -> p h d", d=128), ALU.add, [sq.b], [ssq.b])
            rstd_ops(C, (ssq[:], ssq.b), (lnq[:], lnq.b), (rsq[:], rsq.b), 1.0 / 128)
            TT(C, t1[:].rearrange("p (h d) -> p h d", d=128), bk_o[:].rearrange("p (h d) -> p h d", d=128),
               rsq[:].unsqueeze(2).to_broadcast([128, 4, 128]), ALU.mult, [bk_o.b, rsq.b], [t1.b])
            TT(C, t1[:], t1[:], gain[:], ALU.mult, [t1.b, gain.b], [t1.b])
            TT(C, yb[:], t1[:], gg[:, c, :], ALU.mult, [t1.b, gg.b], [yb.b])
            r0 = cur_b[0] * S + c * 128
            DMA(C, D_["ymix"][r0:r0 + 128, 512:1024], yb[:], reads=[yb.b])

        cur_b = [0]
        for b in range(NSEQ):
            cur_b[0] = b
            DMA(C, qg[:], D_["qgT"][b], writes=[qg.b])
            DMA(C, kg[:], D_["kgT"][b], writes=[kg.b])
            DMA(C, ktok[:], D_["ktok"][b * S:(b + 1) * S, :].rearrange("(i p) f -> p i f", p=128), writes=[ktok.b])
            DMA(C, gv[:], D_["gv"][b * S:(b + 1) * S, :].rearrange("(i p) f -> p i f", p=128), writes=[gv.b])
            DMA(C, gg[:], D_["gg"][b * S:(b + 1) * S, :].rearrange("(i p) f -> p i f", p=128), writes=[gg.b])
            DMA(C, ebl[:], D_["ebl"][b], writes=[ebl.b])
            MS(C, Sf[:], 0.0, [Sf.b])
            MS(C, Sb_[:], 0.0, [Sb_.b])
            for c in range(NT):
                att = atts[c % 2]
                yb = ybs[c % 3]
                cs = slice(c * 128, (c + 1) * 128)
                bk_a = next_bank(C)
                for hh in range(4):
                    MM(C, bk_a[:, hh * 128:(hh + 1) * 128], kg[:, hh, cs], qg[:, hh, cs], True, True, [kg.b, qg.b], [bk_a.b], signal=(hh == 3))
                TT(C, att[:], bk_a[:].rearrange("p (h t) -> p h t", t=128), C.mask01[:].unsqueeze(1).to_broadcast([128, 4, 128]), ALU.mult,
                   [bk_a.b, C.mask01.b], [att.b])
                bk_o = next_bank(C)
                for hh in range(4):
                    o = bk_o[:, hh * 128:(hh + 1) * 128]
                    MM(C, o, qg[:, hh, cs], Sb_[:, hh, :], True, False, [qg.b, Sb_.b], [bk_o.b], signal=False)
                    MM(C, o, att[:, hh, :], gv[:, c, hh * 128:(hh + 1) * 128], False, True, [att.b, gv.b], [bk_o.b], signal=(hh == 3))
                bk_s = next_bank(C)
                for hh in range(4):
                    MM(C, bk_s[0:64, hh * 128:(hh + 1) * 128], ktok[:, c, hh * 64:(hh + 1) * 64], gv[:, c, hh * 128:(hh + 1) * 128], True, True,
                       [ktok.b, gv.b], [bk_s.b], signal=(hh == 3))
                TT(C, tS[:].rearrange("p h v -> p (h v)"), bk_s[0:64, :], Sf[:].rearrange("p h v -> p (h v)"), ALU.add, [bk_s.b, Sf.b], [tS.b])
                TT(C, Sf[:], tS[:], ebl[:, :, c:c + 1].to_broadcast([64, 4, 128]), ALU.mult, [tS.b, ebl.b], [Sf.b])
                CP(C, "act", Sb_[:], Sf[:], [Sf.b], [Sb_.b])
                deferred.append((bk_o, yb, c))
                if len(deferred) > 1:
                    gla_norm(*deferred.pop(0))
            while deferred:
                gla_norm(*deferred.pop(0))
        P.barrier()


def proj_residual(C, l, tagname, ysrc_fn, kchunks, w_dram, hsrc, hdst, ntiles, final_gain=None):
    nc, P = C.nc, C.P
    with contextlib.ExitStack() as ph:
        def sb(name, shape, dt):
            return Tile(ph.enter_context(nc.sbuf_tensor(f"{tagname}{l}_" + name, shape, dt)), name)
        wo = sb("wo", [128, kchunks, DM], BF16)
        wst = [sb(f"wst{i}", [128, DM], F32) for i in range(2)]
        load_weight_bf16(C, wo, [wo[:, k, :] for k in range(kchunks)], [w_dram[k * 128:(k + 1) * 128, :] for k in range(kchunks)], wst)
        yts = [sb(f"yt{i}", [128, kchunks * 128], BF16) for i in range(4)]
        yTs = [sb(f"yT{i}", [128, kchunks, 128], BF16) for i in range(2)]
        hts = [sb(f"ht{i}", [128, DM], F32) for i in range(4)]
        for i in range(ntiles):
            yt, yT, ht = yts[i % 4], yTs[i % 2], hts[i % 4]
            DMA(C, yt[:], ysrc_fn(i), writes=[yt.b])
            DMA(C, ht[:], hsrc[i * 128:(i + 1) * 128, :], writes=[ht.b])
            bank = next_bank(C)
            bv = bf16view(bank)
            for k in range(kchunks):
                TR(C, bv[:, k * 128:(k + 1) * 128], yt[:, k * 128:(k + 1) * 128], C.idb[:], [yt.b, C.idb.b], [bank.b], signal=(k == kchunks - 1))
            CP(C, "act", yT[:], bv[:, 0:kchunks * 128].rearrange("p (k t) -> p k t", k=kchunks), [bank.b], [yT.b])
            for n in range(2):
                bank = next_bank(C)
                mm_group(C, bank[:], bank, [(yT[:, k, :], wo[:, k, n * 512:(n + 1) * 512]) for k in range(kchunks)], [yT.b, wo.b])
                TT(C, ht[:, n * 512:(n + 1) * 512], bank[:], ht[:, n * 512:(n + 1) * 512], ALU.add, [bank.b, ht.b], [ht.b])
            DMA(C, hdst[i * 128:(i + 1) * 128, :], ht[:], reads=[ht.b])
        P.barrier()


def hnext(C):
    return C.D["h2"] if C.hcur is C.D["h"] else C.D["h"]


def phase_B3(C, l):
    dst = hnext(C)
    proj_residual(C, l, "B3", lambda i: C.D["ymix"][i * 128:(i + 1) * 128, :], 8, C.I["w_out"][l], C.hcur, dst, C.T // 128)
    C.hcur = dst


def phase_C(C, l):
    nc, P, I, D_ = C.nc, C.P, C.I, C.D
    S, NSEQ, NG = C.S, C.NSEQ, C.NG
    XS = 128 ** -0.5
    with contextlib.ExitStack() as ph:
        def sb(name, shape, dt):
            return Tile(ph.enter_context(nc.sbuf_tensor(f"C{l}_" + name, shape, dt)), name)
        wst = [sb(f"wst{i}", [128, 4, 512], F32) for i in range(2)]
        wq = sb("wq", [128, 8, 512], BF16)
        wk = sb("wk", [128, 8, 512], BF16)
        wv = sb("wv", [128, 8, 512], BF16)
        for wt, name in ((wk, "w_xk"), (wv, "w_xv"), (wq, "w_xq")):
            src = I[name][l].rearrange("(k p) f -> p k f", p=128)
            load_weight_bf16(C, wt, [wt[:, 0:4, :], wt[:, 4:8, :]], [src[:, 0:4, :], src[:, 4:8, :]], wst)
        gain = sb("gain", [128, DM], F32)
        DMA(C, gain[:], I["cross_norm"][l].partition_broadcast(128), writes=[gain.b])
        KmT = sb("KmT", [128, NSEQ, 4, MEM], BF16)
        Vm = sb("Vm", [128, NSEQ, 2, 4, 129], BF16)
        MS(C, Vm[:], 1.0, [Vm.b])
        for b in range(NSEQ):
            for hh in range(4):
                bank = next_bank(C)
                mm_group(C, bank[:, 0:MEM], bank, [(wk[:, k, hh * 128:(hh + 1) * 128], C.memnT[:, k, b * MEM:(b + 1) * MEM]) for k in range(8)],
                         [wk.b, C.memnT.b])
                CP(C, "act", KmT[:, b, hh, :], bank[:, 0:MEM], [bank.b], [KmT.b], scale=XS)
            for mb in range(2):
                bank = next_bank(C)
                mm_group(C, bank[:], bank, [(C.memnT[:, k, b * MEM + mb * 128:b * MEM + (mb + 1) * 128], wv[:, k, :]) for k in range(8)],
                         [wv.b, C.memnT.b])
                CP(C, "dve", Vm[:, b, mb, :, 0:128], bank[:].rearrange("p (h d) -> p h d", d=128), [bank.b], [Vm.b])
        xts, junk, hns, smalls = norm_scratch(sb)
        hnTs = [sb(f"hnT{i}", [128, 8, 512], BF16) for i in range(2)]
        qTs = [sb(f"qT{i}", [128, 4, 512], BF16) for i in range(2)]
        PTs = [sb(f"PT{i}", [128, 2, 512], BF16) for i in range(2)]
        obs = [sb(f"ob{i}", [128, 4, 512], BF16) for i in range(2)]
        recs = [sb(f"rec{i}", [128, 1], F32) for i in range(2)]
        P.barrier()
        cstop = C.cfg.get("cstop", 9)
        gi = pi = ri = 0
        for b in range(NSEQ):
            if cstop <= 1:
                break
            for g in range(NG):
                hnT, qT, ob = hnTs[gi % 2], qTs[gi % 2], obs[gi % 2]
                gi += 1
                tg = b * S + g * 512
                for i in range(4):
                    t0 = tg + i * 128
                    norm_transpose_tile(C, C.hcur[t0:t0 + 128, :], xts[i % 2], junk, smalls[i % 2], hns[i % 2], gain, hnT, i * 128)
                for hh in range(4):
                    bank = next_bank(C)
                    mm_group(C, bank[:], bank, [(wq[:, k, hh * 128:(hh + 1) * 128], hnT[:, k, :]) for k in range(8)], [wq.b, hnT.b])
                    CP(C, "act" if hh % 2 == 0 else "dve", qT[:, hh, :], bank[:], [bank.b], [qT.b])
                for hh in range(4):
                    if cstop <= 2:
                        break
                    PT = PTs[pi % 2]
                    pi += 1
                    for mb in range(2):
                        bank = next_bank(C)
                        MM(C, bank[:], KmT[:, b, hh, mb * 128:(mb + 1) * 128], qT[:, hh, :], True, True, [KmT.b, qT.b], [bank.b])
                        ACT(C, PT[:, mb, :], bank[:], AF.Exp, [bank.b], [PT.b])
                    for half in range(2):
                        if cstop <= 3:
                            break
                        bank = next_bank(C)
                        for ii in range(2):
                            i = half * 2 + ii
                            o = bank[:, ii * 256:ii * 256 + 129]
                            for mb in range(2):
                                MM(C, o, PT[:, mb, i * 128:(i + 1) * 128], Vm[:, b, mb, hh, :], mb == 0, mb == 1, [PT.b, Vm.b], [bank.b],
                                   signal=(mb == 1 and ii == 1))
                        for ii in range(2):
                            i = half * 2 + ii
                            rec = recs[ri % 2]
                            ri += 1
                            RCP(C, rec[:], bank[:, ii * 256 + 128:ii * 256 + 129], [bank.b], [rec.b])
                            TS(C, ob[:, i, hh * 128:(hh + 1) * 128], bank[:, ii * 256:ii * 256 + 128], rec[:], None, ALU.mult, None,
                               [bank.b, rec.b], [ob.b])
                DMA(C, D_["ymix"][tg:tg + 512, 0:512].rearrange("(i p) f -> p i f", p=128), ob[:], reads=[ob.b])
        P.barrier()
    if C.cfg.get("cstop", 9) <= 4:
        return
    dst = hnext(C)
    proj_residual(C, l, "C3", lambda i: D_["ymix"][i * 128:(i + 1) * 128, 0:512], 4, I["w_xo"][l], C.hcur, dst, C.T // 128)
    C.hcur = dst


BIG = 10000.0


def phase_D(C, l, last):
    nc, P, I, D_ = C.nc, C.P, C.I, C.D
    S, NSEQ, NG, NT = C.S, C.NSEQ, C.NG, C.NT
    if C.cfg.get("nofinal"):
        last = False
    with contextlib.ExitStack() as ph:
        def sb(name, shape, dt):
            return Tile(ph.enter_context(nc.sbuf_tensor(f"D{l}_" + name, shape, dt)), name)
        gain = sb("gain", [128, DM], F32)
        DMA(C, gain[:], I["moe_norm"][l].partition_broadcast(128), writes=[gain.b])
        Wr = sb("Wr", [128, 8, 20], F32)
        DMA(C, Wr[:, :, 0:4], I["w_rg"][l].rearrange("(k p) f -> p k f", p=128), writes=[Wr.b], allow_slow_non_contiguous=True)
        DMA(C, Wr[:, :, 4:20], I["w_re"][l].rearrange("(k p) f -> p k f", p=128), writes=[Wr.b], allow_slow_non_contiguous=True)
        Wrh = sb("Wrh", [128, 8, 20], BF16)
        Wrl = sb("Wrl", [128, 8, 20], BF16)
        CP(C, "dve", Wrh[:], Wr[:], [Wr.b], [Wrh.b])
        TT(C, Wrl[:], Wr[:], Wrh[:], ALU.subtract, [Wr.b, Wrh.b], [Wrl.b])
        brb = sb("brb", [128, 20], F32)
        DMA(C, brb[:, 0:4], I["b_rg"][l].partition_broadcast(128), writes=[brb.b])
        DMA(C, brb[:, 4:20], I["b_re"][l].partition_broadcast(128), writes=[brb.b])
        hnT = sb("hnT", [128, 8, S], BF16)
        acc = sb("acc", [128, NT, DM], F32)
        lg = sb("lg", [128, NT, 20], F32)
        G = sb("G", [128, NT, 16], F32)
        if last:
            fgain = sb("fgain", [128, DM], F32)
            DMA(C, fgain[:], I["final_norm"].partition_broadcast(128), writes=[fgain.b])
        P.barrier()
        hdst = hnext(C)
        for b in range(NSEQ):
            with contextlib.ExitStack() as ph2:
                def sb2(name, shape, dt):
                    return Tile(ph2.enter_context(nc.sbuf_tensor(f"D{l}r{b}_" + name, shape, dt)), name)
                xts = [sb2(f"xt{i}", [128, DM], F32) for i in range(2)]
                junk = sb2("junk", [128, DM], BF16)
                hn32s = [sb2(f"hn32{i}", [128, DM], F32) for i in range(2)]
                his = [sb2(f"hi{i}", [128, DM], BF16) for i in range(2)]
                los = [sb2(f"lo{i}", [128, DM], BF16) for i in range(2)]
                loTs = [sb2(f"loT{i}", [128, 8, 128], BF16) for i in range(2)]
                smalls = [[sb2(f"{n}{i}", [128, 1], F32) for n in ("ss", "ln", "rstd")] for i in range(2)]
                sm = {n: sb2(n, [128, NT], F32) for n in ("gmax", "sg", "pg", "m1", "m2", "den", "coef")}
                g4 = {n: sb2(n, [128, NT, 4], F32) for n in ("gm", "eg", "pen")}
                e16 = {n: sb2(n, [128, NT, 16], F32) for n in ("lem", "isf", "le2", "sel", "w")}
                P.barrier()
                for i in range(NT):
                    t0 = b * S + i * 128
                    xt, hn32, hi_, lo_, loT = xts[i % 2], hn32s[i % 2], his[i % 2], los[i % 2], loTs[i % 2]
                    norm_tile(C, C.hcur[t0:t0 + 128, :], xt, junk, smalls[i % 2], gain, hn32[:], hn32.b)
                    CP(C, "act", hi_[:], hn32[:], [hn32.b], [hi_.b])
                    TT(C, lo_[:], hn32[:], hi_[:], ALU.subtract, [hn32.b, hi_.b], [lo_.b])
                    tsl = slice(i * 128, (i + 1) * 128)
                    bank = next_bank(C)
                    bv = bf16view(bank)
                    for k in range(8):
                        TR(C, bv[:, k * 128:(k + 1) * 128], hi_[:, k * 128:(k + 1) * 128], C.idb[:], [hi_.b, C.idb.b], [bank.b], signal=(k == 7))
                    CP(C, "act", hnT[:, :, tsl], bv.rearrange("p (k t) -> p k t", k=8), [bank.b], [hnT.b])
                    bank = next_bank(C)
                    bv = bf16view(bank)
                    for k in range(8):
                        TR(C, bv[:, k * 128:(k + 1) * 128], lo_[:, k * 128:(k + 1) * 128], C.idb[:], [lo_.b, C.idb.b], [bank.b], signal=(k == 7))
                    CP(C, "dve", loT[:], bv.rearrange("p (k t) -> p k t", k=8), [bank.b], [loT.b])
                    bank = next_bank(C)
                    pairs = []
                    for k in range(8):
                        pairs += [(hnT[:, k, tsl], Wrh[:, k, :]), (loT[:, k, :], Wrh[:, k, :]), (hnT[:, k, tsl], Wrl[:, k, :])]
                    mm_group(C, bank[:, 0:20], bank, pairs, [hnT.b, loT.b, Wrh.b, Wrl.b])
                    TT(C, lg[:, i, :], bank[:, 0:20], brb[:], ALU.add, [bank.b, brb.b], [lg.b])
                gl = lg[:, :, 0:4]
                el = lg[:, :, 4:20]
                bc4 = lambda t_: t_[:].unsqueeze(2).to_broadcast([128, NT, 4])
                bc16 = lambda t_: t_[:].unsqueeze(2).to_broadcast([128, NT, 16])
                RED(C, sm["gmax"][:], gl, ALU.max, [lg.b], [sm["gmax"].b])
                TT(C, g4["gm"][:], gl, bc4(sm["gmax"]), ALU.is_ge, [lg.b, sm["gmax"].b], [g4["gm"].b])
                TT(C, g4["eg"][:], gl, bc4(sm["gmax"]), ALU.subtract, [lg.b, sm["gmax"].b], [g4["eg"].b])
                ACT(C, g4["eg"][:], g4["eg"][:], AF.Exp, [g4["eg"].b], [g4["eg"].b])
                RED(C, sm["sg"][:], g4["eg"][:], ALU.add, [g4["eg"].b], [sm["sg"].b])
                RCP(C, sm["pg"][:], sm["sg"][:], [sm["sg"].b], [sm["pg"].b])
                TS(C, g4["pen"][:], g4["gm"][:], BIG, -BIG, ALU.mult, ALU.add, [g4["gm"].b], [g4["pen"].b])
                for gq_ in range(4):
                    TT(C, e16["lem"][:, :, gq_ * 4:(gq_ + 1) * 4], lg[:, :, 4 + gq_ * 4:8 + gq_ * 4],
                       g4["pen"][:, :, gq_:gq_ + 1].to_broadcast([128, NT, 4]), ALU.add, [lg.b, g4["pen"].b], [e16["lem"].b])
                RED(C, sm["m1"][:], e16["lem"][:], ALU.max, [e16["lem"].b], [sm["m1"].b])
                TT(C, e16["isf"][:], e16["lem"][:], bc16(sm["m1"]), ALU.is_ge, [e16["lem"].b, sm["m1"].b], [e16["isf"].b])
                STT(C, e16["le2"][:], e16["isf"][:], -BIG, e16["lem"][:], ALU.mult, ALU.add, [e16["isf"].b, e16["lem"].b], [e16["le2"].b])
                RED(C, sm["m2"][:], e16["le2"][:], ALU.max, [e16["le2"].b], [sm["m2"].b])
                TT(C, e16["sel"][:], e16["lem"][:], bc16(sm["m2"]), ALU.is_ge, [e16["lem"].b, sm["m2"].b], [e16["sel"].b])
                TT(C, e16["w"][:], e16["lem"][:], bc16(sm["m1"]), ALU.subtract, [e16["lem"].b, sm["m1"].b], [e16["w"].b])
                ACT(C, e16["w"][:], e16["w"][:], AF.Exp, [e16["w"].b], [e16["w"].b])
                TT(C, e16["w"][:], e16["w"][:], e16["sel"][:], ALU.mult, [e16["w"].b, e16["sel"].b], [e16["w"].b])
                RED(C, sm["den"][:], e16["w"][:], ALU.add, [e16["w"].b], [sm["den"].b])
                RCP(C, sm["den"][:], sm["den"][:], [sm["den"].b], [sm["den"].b])
                TT(C, sm["coef"][:], sm["den"][:], sm["pg"][:], ALU.mult, [sm["den"].b, sm["pg"].b], [sm["coef"].b])
                TT(C, G[:], e16["w"][:], bc16(sm["coef"]), ALU.mult, [e16["w"].b, sm["coef"].b], [G.b])
                P.barrier()
            if C.cfg.get("dstop", 9) <= 1:
                continue
            with contextlib.ExitStack() as ph3:
                def sb3(name, shape, dt):
                    return Tile(ph3.enter_context(nc.sbuf_tensor(f"D{l}e{b}_" + name, shape, dt)), name)
                wst = [sb3(f"wst{i}", [128, 2048], F32) for i in range(2)]
                Wgs = [sb3(f"Wg{i}", [128, 8, DEXP], BF16) for i in range(2)]
                Wus = [sb3(f"Wu{i}", [128, 8, DEXP], BF16) for i in range(2)]
                Wds = [sb3(f"Wd{i}", [128, 4, DM], BF16) for i in range(2)]
                uTs = [sb3(f"uT{i}", [128, 4, 512], BF16) for i in range(2)]
                sgs = [sb3(f"sg{i}", [128, 512], BF16) for i in range(2)]
                P.barrier()
                ui = 0
                for e in range(NEXP):
                    Wg, Wu, Wd = Wgs[e % 2], Wus[e % 2], Wds[e % 2]
                    sg_ = I["w_eg"][l, e].rearrange("(k p) f -> p k f", p=128)
                    su_ = I["w_eu"][l, e].rearrange("(k p) f -> p k f", p=128)
                    sd_ = I["w_ed"][l, e].rearrange("(k p) f -> p k f", p=128)
                    load_weight_bf16(C, Wg, [Wg[:, 0:4, :], Wg[:, 4:8, :]], [sg_[:, 0:4, :], sg_[:, 4:8, :]], wst)
                    load_weight_bf16(C, Wu, [Wu[:, 0:4, :], Wu[:, 4:8, :]], [su_[:, 0:4, :], su_[:, 4:8, :]], wst)
                    load_weight_bf16(C, Wd, [Wd[:, 0:2, :], Wd[:, 2:4, :]], [sd_[:, 0:2, :], sd_[:, 2:4, :]], wst)
                    for g in range(NG):
                        uT = uTs[ui % 2]
                        ui += 1
                        xs = hnT[:, :, g * 512:(g + 1) * 512]
                        for fc in range(4):
                            sg = sgs[fc % 2]
                            bg = next_bank(C)
                            mm_group(C, bg[:], bg, [(Wg[:, k, fc * 128:(fc + 1) * 128], hnT[:, k, g * 512:(g + 1) * 512]) for k in range(8)],
                                     [Wg.b, hnT.b])
                            ACT(C, sg[:], bg[:], AF.Silu, [bg.b], [sg.b])
                            bu = next_bank(C)
                            mm_group(C, bu[:], bu, [(Wu[:, k, fc * 128:(fc + 1) * 128], hnT[:, k, g * 512:(g + 1) * 512]) for k in range(8)],
                                     [Wu.b, hnT.b])
                            TT(C, uT[:, fc, :], bu[:], sg[:], ALU.mult, [bu.b, sg.b], [uT.b])
                        for i in range(4):
                            ti = g * 4 + i
                            for n in range(2):
                                by = next_bank(C)
                                mm_group(C, by[:], by, [(uT[:, fc, i * 128:(i + 1) * 128], Wd[:, fc, n * 512:(n + 1) * 512]) for fc in range(4)],
                                         [uT.b, Wd.b])
                                a = acc[:, ti, n * 512:(n + 1) * 512]
                                if e == 0:
                                    TS(C, a, by[:], G[:, ti, e:e + 1], None, ALU.mult, None, [by.b, G.b], [acc.b])
                                else:
                                    STT(C, a, by[:], G[:, ti, e:e + 1], a, ALU.mult, ALU.add, [by.b, G.b, acc.b], [acc.b])
                P.barrier()
            if C.cfg.get("dstop", 9) <= 2:
                continue
            with contextlib.ExitStack() as ph4:
                def sb4(name, shape, dt):
                    return Tile(ph4.enter_context(nc.sbuf_tensor(f"D{l}f{b}_" + name, shape, dt)), name)
                hts = [sb4(f"ht{i}", [128, DM], F32) for i in range(2)]
                ots = [sb4(f"ot{i}", [128, DM], F32) for i in range(2)]
                junk = sb4("junk", [128, DM], BF16)
                smalls = [[sb4(f"{n}{i}", [128, 1], F32) for n in ("ss", "ln", "rstd")] for i in range(2)]
                P.barrier()
                for i in range(NT):
                    t0 = b * S + i * 128
                    ht = hts[i % 2]
                    ss, ln, rstd = smalls[i % 2]
                    DMA(C, ht[:], C.hcur[t0:t0 + 128, :], writes=[ht.b])
                    TT(C, ht[:], ht[:], acc[:, i, :], ALU.add, [ht.b, acc.b], [ht.b])
                    if not last:
                        DMA(C, hdst[t0:t0 + 128, :], ht[:], reads=[ht.b])
                    else:
                        ACT(C, junk[:], ht[:], AF.Square, [ht.b], [junk.b, ss.b], accum_out=ss[:])
                        rstd_ops(C, (ss[:], ss.b), (ln[:], ln.b), (rstd[:], rstd.b), 1.0 / DM)
                        ot = ots[i % 2]
                        STT(C, ot[:], ht[:], rstd[:], fgain[:], ALU.mult, ALU.mult, [ht.b, rstd.b, fgain.b], [ot.b])
                        odst = hdst if C.cfg.get("out_to_h") else C.out
                        DMA(C, odst[t0:t0 + 128, :], ot[:], reads=[ot.b])
                P.barrier()
        P.barrier()
        C.hcur = hdst


def consts():
    s = np.arange(128)[:, None]
    t = np.arange(128)[None, :]
    mq = np.zeros((128, 4, 512), np.float32)
    for r in range(4):
        for cb in range(4):
            blk = mq[:, r, cb * 128:(cb + 1) * 128]
            if cb < r:
                blk[:] = NEG
            elif cb == r:
                blk[:] = np.where(s <= t, 0.0, NEG)
    return {
        "c_maskq": mq.astype(ml_dtypes.bfloat16),
        "c_ident": np.eye(128, dtype=np.float32),
        "c_maskadd": np.where(s <= t, 0.0, NEG).astype(np.float32),
        "c_mask01": (s <= t).astype(np.float32),
    }


_RENAME = {"w_rg": "w_router_group", "b_rg": "b_router_group", "w_re": "w_router_expert", "b_re": "b_router_expert",
           "w_eg": "w_expert_gate", "w_eu": "w_expert_up", "w_ed": "w_expert_down"}
N_CORES = 8


def kernel(**inputs):
    x = np.asarray(inputs["x"], dtype=np.float32)
    mem = np.asarray(inputs["mem"], dtype=np.float32)
    B, S, _ = x.shape
    L = int(np.asarray(inputs["w_in"]).shape[0])
    NSEQ = B // N_CORES
    nc, C = build(dict(S=S, NSEQ=NSEQ, L=L, phases="0ABCD", debug=False))
    shared = {k: np.ascontiguousarray(np.asarray(v, dtype=np.float32)) for k, v in inputs.items() if k not in ("x", "mem")}
    shared.update(consts())
    in_maps = []
    for c in range(N_CORES):
        m = dict(shared)
        m["x"] = np.ascontiguousarray(x[c * NSEQ:(c + 1) * NSEQ].reshape(NSEQ * S, DM))
        m["mem"] = np.ascontiguousarray(mem[c * NSEQ:(c + 1) * NSEQ].reshape(NSEQ * MEM, DM))
        in_maps.append(m)
    res = run_bass_kernel_spmd(nc, in_maps, core_ids=list(range(N_CORES)))
    outs = [np.asarray(r["y_out"]).reshape(NSEQ, S, DM) for r in res.results]
    return np.concatenate(outs, axis=0).astype(np.float32)
```
